# Optimizing a Trainium2 kernel written in Bass

```python
import math
import jax, jax.numpy as jnp
from jax import lax
import numpy as np

D_MODEL = 1024
BATCH = 4
SEQ = 8192
DEPTH = 1

RET_HEADS = 4
RET_HEAD_DIM = 128
RET_WIDTH = RET_HEADS * RET_HEAD_DIM
RET_CHUNK = 256
MOBA_HEADS = 8
MOBA_HEAD_DIM = 64
MOBA_WIDTH = MOBA_HEADS * MOBA_HEAD_DIM
MOBA_BLOCK = 256
MOBA_TOP_K = 3
MOBA_Q_CHUNK = 64
MIX_WIDTH = RET_WIDTH + MOBA_WIDTH
IN_WIDTH = 4 * RET_WIDTH + 3 * MOBA_WIDTH
D_FF = ((8 * D_MODEL + 3 * 256 - 1) // (3 * 256)) * 256
DEEPNORM_ALPHA = (2 * DEPTH) ** 0.25
DEEPNORM_BETA = (8 * DEPTH) ** -0.25
NORM_EPS = 1e-5
NEG_INF = -1e30
SEQ_PAD_MULTIPLE = math.lcm(RET_CHUNK, MOBA_BLOCK)

kernel_name = "hybrid_retention_moba_deepnorm"


def _layer_norm(x, g, b):
    xf = x.astype(jnp.float32)
    mu = jnp.mean(xf, axis=-1, keepdims=True)
    var = jnp.mean(jnp.square(xf - mu), axis=-1, keepdims=True)
    return ((xf - mu) * lax.rsqrt(var + NORM_EPS) * g + b).astype(x.dtype)


def _split_in_proj(p):
    sizes = [RET_WIDTH, RET_WIDTH, RET_WIDTH, RET_WIDTH, MOBA_WIDTH, MOBA_WIDTH, MOBA_WIDTH]
    offs = np.cumsum([0] + sizes)
    return [p[..., int(offs[i]):int(offs[i + 1])] for i in range(len(sizes))]


def _retention(q, k, v):
    f32 = jnp.float32
    bsz, s_len, n_h, d_k = q.shape
    d_v = v.shape[-1]
    n_c = s_len // RET_CHUNK
    q = q.astype(f32).reshape(bsz, n_c, RET_CHUNK, n_h, d_k)
    k = (k.astype(f32) * d_k ** -0.5).reshape(bsz, n_c, RET_CHUNK, n_h, d_k)
    v = v.astype(f32).reshape(bsz, n_c, RET_CHUNK, n_h, d_v)
    log_g = jnp.log1p(-jnp.exp2(-5.0 - jnp.arange(n_h, dtype=f32)))
    pos = jnp.arange(RET_CHUNK, dtype=f32)
    diff = pos[:, None] - pos[None, :]
    intra_decay = jnp.where(diff >= 0.0,
                            jnp.exp(log_g[:, None, None] * jnp.maximum(diff, 0.0)), 0.0)
    scores = jnp.einsum('bnihd,bnjhd->bnhij', q, k) * intra_decay
    intra = jnp.einsum('bnhij,bnjhe->bnihe', scores, v)
    k_to_end = jnp.exp(log_g[None, :] * (RET_CHUNK - 1.0 - pos)[:, None])
    kv = jnp.einsum('bnjhd,bnjhe->nbhde', k * k_to_end[:, :, None], v)
    chunk_decay = jnp.exp(log_g * RET_CHUNK)[None, :, None, None]

    def step(state, kv_c):
        return state * chunk_decay + kv_c, state

    _, prev = lax.scan(step, jnp.zeros_like(kv[0]), kv)
    q_from_start = jnp.exp(log_g[None, :] * (pos + 1.0)[:, None])
    cross = jnp.einsum('bnihd,nbhde->bnihe', q * q_from_start[:, :, None], prev)
    return (intra + cross).reshape(bsz, s_len, n_h, d_v)


def _head_group_norm(y, gain):
    bsz, s_len, n_h, e = y.shape
    mu = jnp.mean(y, axis=-1, keepdims=True)
    var = jnp.mean(jnp.square(y - mu), axis=-1, keepdims=True)
    yn = (y - mu) * lax.rsqrt(var + NORM_EPS)
    return yn.reshape(bsz, s_len, n_h * e) * gain


def _alibi_slopes(n_heads):
    return jnp.exp2(-8.0 * (jnp.arange(n_heads, dtype=jnp.float32) + 1.0) / n_heads)


def _moba_attention(q, k, v):
    f32 = jnp.float32
    bsz, s_len, n_h, d_h = q.shape
    n_b = s_len // MOBA_BLOCK
    top_k = min(MOBA_TOP_K, n_b)
    q = jnp.transpose(q, (0, 2, 1, 3)).astype(f32) * d_h ** -0.5
    k = jnp.transpose(k, (0, 2, 1, 3)).astype(f32)
    v = jnp.transpose(v, (0, 2, 1, 3)).astype(f32)
    kb = k.reshape(bsz, n_h, n_b, MOBA_BLOCK, d_h)
    vb = v.reshape(bsz, n_h, n_b, MOBA_BLOCK, d_h)
    k_mean = jnp.mean(kb, axis=3)
    slopes = _alibi_slopes(n_h)
    b_idx = jnp.arange(bsz)[:, None, None, None]
    h_idx = jnp.arange(n_h)[None, :, None, None]
    blk_ids = jnp.arange(n_b)
    offs = jnp.arange(MOBA_BLOCK)

    def one_chunk(start):
        qc = lax.dynamic_slice_in_dim(q, start, MOBA_Q_CHUNK, axis=2)
        t_q = start + jnp.arange(MOBA_Q_CHUNK)
        own = start // MOBA_BLOCK
        gate = jnp.einsum('bhqd,bhnd->bhqn', qc, k_mean)
        gate = jnp.where(blk_ids < own, gate, NEG_INF)
        _, sel = lax.top_k(gate, top_k)
        sel_valid = jnp.arange(top_k) < own
        k_sel = kb[b_idx, h_idx, sel]
        v_sel = vb[b_idx, h_idx, sel]
        dist_sel = (t_q[None, None, :, None, None]
                    - (sel[..., None] * MOBA_BLOCK + offs)).astype(f32)
        s_sel = (jnp.einsum('bhqd,bhqkjd->bhqkj', qc, k_sel)
                 - slopes[:, None, None, None] * dist_sel)
        s_sel = jnp.where(sel_valid[:, None], s_sel, NEG_INF)
        k_own = lax.dynamic_slice_in_dim(k, own * MOBA_BLOCK, MOBA_BLOCK, axis=2)
        v_own = lax.dynamic_slice_in_dim(v, own * MOBA_BLOCK, MOBA_BLOCK, axis=2)
        dist_own = t_q[:, None] - (own * MOBA_BLOCK + offs)[None, :]
        s_own = (jnp.einsum('bhqd,bhjd->bhqj', qc, k_own)
                 - slopes[:, None, None] * dist_own.astype(f32))
        s_own = jnp.where(dist_own >= 0, s_own, NEG_INF)
        logits = jnp.concatenate(
            [s_sel.reshape(bsz, n_h, MOBA_Q_CHUNK, top_k * MOBA_BLOCK), s_own], axis=-1)
        p = jax.nn.softmax(logits, axis=-1)
        p_sel = p[..., :top_k * MOBA_BLOCK].reshape(bsz, n_h, MOBA_Q_CHUNK, top_k, MOBA_BLOCK)
        p_own = p[..., top_k * MOBA_BLOCK:]
        return (jnp.einsum('bhqkj,bhqkjd->bhqd', p_sel, v_sel)
                + jnp.einsum('bhqj,bhjd->bhqd', p_own, v_own))

    starts = jnp.arange(0, s_len, MOBA_Q_CHUNK)
    out = lax.map(one_chunk, starts)
    return jnp.transpose(out, (1, 0, 3, 2, 4)).reshape(bsz, s_len, n_h, d_h)


def setup_inputs(seed: int = 0) -> dict:
    key = jax.random.key(seed)
    ks = jax.random.split(key, 12)
    f32 = jnp.float32
    nrm = lambda k_, shp: jax.random.normal(k_, shp, f32)
    col_scale = jnp.concatenate([
        jnp.ones((2 * RET_WIDTH,), f32), jnp.full((RET_WIDTH,), DEEPNORM_BETA, f32),
        jnp.ones((RET_WIDTH,), f32), jnp.ones((2 * MOBA_WIDTH,), f32),
        jnp.full((MOBA_WIDTH,), DEEPNORM_BETA, f32)])
    x = nrm(ks[0], (BATCH, SEQ, D_MODEL))
    w_in = nrm(ks[1], (DEPTH, D_MODEL, IN_WIDTH)) * (D_MODEL ** -0.5) * col_scale
    ret_gn_gain = 1.0 + 0.02 * nrm(ks[2], (DEPTH, RET_WIDTH))
    w_out = nrm(ks[3], (DEPTH, MIX_WIDTH, D_MODEL)) * (MIX_WIDTH ** -0.5) * DEEPNORM_BETA
    ln1_g = 1.0 + 0.02 * nrm(ks[4], (DEPTH, D_MODEL))
    ln1_b = 0.02 * nrm(ks[5], (DEPTH, D_MODEL))
    w_gate = nrm(ks[6], (DEPTH, D_MODEL, D_FF)) * (D_MODEL ** -0.5) * DEEPNORM_BETA
    w_up = nrm(ks[7], (DEPTH, D_MODEL, D_FF)) * (D_MODEL ** -0.5) * DEEPNORM_BETA
    w_down = nrm(ks[8], (DEPTH, D_FF, D_MODEL)) * (D_FF ** -0.5) * DEEPNORM_BETA
    ln2_g = 1.0 + 0.02 * nrm(ks[9], (DEPTH, D_MODEL))
    ln2_b = 0.02 * nrm(ks[10], (DEPTH, D_MODEL))
    return {"x": x, "w_in": w_in, "ret_gn_gain": ret_gn_gain, "w_out": w_out,
            "ln1_g": ln1_g, "ln1_b": ln1_b, "w_gate": w_gate, "w_up": w_up,
            "w_down": w_down, "ln2_g": ln2_g, "ln2_b": ln2_b}


def reference(x, w_in, ret_gn_gain, w_out, ln1_g, ln1_b, w_gate, w_up, w_down, ln2_g, ln2_b):
    bsz, s_len, _ = x.shape
    s_pad = -(-s_len // SEQ_PAD_MULTIPLE) * SEQ_PAD_MULTIPLE
    h = x
    for layer in range(DEPTH):
        proj = h @ w_in[layer]
        proj_p = jnp.pad(proj, ((0, 0), (0, s_pad - s_len), (0, 0)))
        rq, rk, rv, rg, mq, mk, mv = _split_in_proj(proj_p)
        ret = _retention(rq.reshape(bsz, s_pad, RET_HEADS, RET_HEAD_DIM),
                         rk.reshape(bsz, s_pad, RET_HEADS, RET_HEAD_DIM),
                         rv.reshape(bsz, s_pad, RET_HEADS, RET_HEAD_DIM))[:, :s_len]
        ret = _head_group_norm(ret, ret_gn_gain[layer])
        ret = jax.nn.silu(rg[:, :s_len].astype(jnp.float32)) * ret
        moba = _moba_attention(mq.reshape(bsz, s_pad, MOBA_HEADS, MOBA_HEAD_DIM),
                               mk.reshape(bsz, s_pad, MOBA_HEADS, MOBA_HEAD_DIM),
                               mv.reshape(bsz, s_pad, MOBA_HEADS, MOBA_HEAD_DIM))[:, :s_len]
        moba = moba.reshape(bsz, s_len, MOBA_WIDTH)
        mixed = jnp.concatenate([ret, moba], axis=-1).astype(h.dtype) @ w_out[layer]
        h = _layer_norm(DEEPNORM_ALPHA * h + mixed, ln1_g[layer], ln1_b[layer])
        ffn = (jax.nn.silu(h @ w_gate[layer]) * (h @ w_up[layer])) @ w_down[layer]
        h = _layer_norm(DEEPNORM_ALPHA * h + ffn, ln2_g[layer], ln2_b[layer])
    return h
```

```python
import numpy as np
import ml_dtypes
from contextlib import ExitStack
import concourse.bass as bass
import concourse.mybir as mybir
from concourse.bass_utils import run_bass_kernel_spmd

F32 = mybir.dt.float32
BF16 = mybir.dt.bfloat16
AF = mybir.ActivationFunctionType
ALU = mybir.AluOpType

D = 1024
DFF = 2816
NFT = DFF // 128
BIG = 30000.0
ALPHA = 2.0 ** 0.25
EPS = 1e-5
ENG = ("pe", "act", "dve", "pool", "sp")
SAME_ENGINE_SYNC = False


class Sem:
    def __init__(self):
        self.h = None
        self.count = 0


class Buf:
    def __init__(self, name, excl=False):
        self.name = name
        self.excl = excl
        self.w = {}
        self.r = {}
        self.dsem = None


class Prog:
    def __init__(self, nc):
        self.nc = nc
        self.ops = []
        self.q = {e: [] for e in ENG}
        self.dsems = []

    def _deps(self, eng, reads, writes, own=None):
        deps = {}

        def add(k, v, is_write):
            if isinstance(k, tuple):
                if k[1] is own:
                    return
                deps[k] = max(deps.get(k, 0), v)
            else:
                if k == eng and (eng == "pe" or not is_write):
                    return
                deps[k] = max(deps.get(k, -1), v)

        for b in reads:
            for k, v in b.w.items():
                add(k, v, True)
        for b in writes:
            for k, v in b.w.items():
                add(k, v, True)
            for k, v in b.r.items():
                add(k, v, False)
        return deps

    def _push(self, eng, fn, deps, dma=None):
        for k, v in deps.items():
            if not isinstance(k, tuple):
                self.ops[v]["signal"] = True
        idx = len(self.ops)
        self.ops.append(dict(eng=eng, fn=fn, deps=deps, signal=False, dma=dma, sid=None))
        self.q[eng].append(idx)
        return idx

    def op(self, eng, fn, reads=(), writes=()):
        ex = [b for b in reads if b.excl]
        reads = [b for b in reads if not b.excl]
        writes = list(writes) + ex
        deps = self._deps(eng, reads, writes)
        idx = self._push(eng, fn, deps)
        for b in reads:
            b.r[eng] = idx
        for b in writes:
            b.w[eng] = idx
        return idx

    def dma(self, queue, out_ap, in_ap, reads=(), writes=(), semb=None):
        b0 = semb or (writes[0] if writes else reads[0])
        if b0.dsem is None:
            b0.dsem = Sem()
            self.dsems.append(b0.dsem)
        s = b0.dsem
        deps = self._deps(None, list(reads), list(writes), own=s)
        s.count += 16
        val = s.count
        self._push(queue, lambda e: e.dma_start(out=out_ap, in_=in_ap), deps, dma=(s, val))
        for b in reads:
            b.r[("dma", s)] = val
        for b in writes:
            b.w[("dma", s)] = val

    def barrier(self):
        last = {}
        for e in ENG:
            for idx in reversed(self.q[e]):
                o = self.ops[idx]
                if o["fn"] is not None and o["dma"] is None:
                    last[e] = idx
                    break
        for e in ENG:
            deps = {}
            for k, idx in last.items():
                if k != e:
                    deps[k] = idx
            for s in self.dsems:
                if s.count:
                    deps[("dma", s)] = s.count
            self._push(e, None, deps)

    def emit(self):
        nc = self.nc
        SEG = 20000
        cnt = {e: 0 for e in ENG}
        for o in self.ops:
            if o["signal"]:
                cnt[o["eng"]] += 1
                o["sid"] = cnt[o["eng"]]
        with ExitStack() as es:
            esem = {}
            for e in ENG:
                esem[e] = [es.enter_context(nc.semaphore(f"s_{e}_{i}")) for i in range(cnt[e] // SEG + 1)]
            for i, s in enumerate(self.dsems):
                s.h = es.enter_context(nc.semaphore(f"d{i}"))
            block = es.enter_context(nc.Block())

            def run(name):
                def f(e):
                    waited = {}
                    for idx in self.q[name]:
                        o = self.ops[idx]
                        for k, v in o["deps"].items():
                            if isinstance(k, tuple):
                                sem, val, key = k[1].h, v, ("d", id(k[1]))
                            else:
                                sid = self.ops[v]["sid"]
                                seg = (sid - 1) // SEG
                                sem, val, key = esem[k][seg], sid - seg * SEG, (k, seg)
                            if waited.get(key, 0) >= val:
                                continue
                            waited[key] = val
                            e.wait_ge(sem, val)
                        if o["fn"] is None:
                            continue
                        ins = o["fn"](e)
                        if o["dma"] is not None:
                            ins.then_inc(o["dma"][0].h, 16)
                        elif o["signal"]:
                            seg = (o["sid"] - 1) // SEG
                            ins.then_inc(esem[name][seg], 1)
                return f

            block.tensor(run("pe"))
            block.scalar(run("act"))
            block.vector(run("dve"))
            block.gpsimd(run("pool"))
            block.sync(run("sp"))


def _bf(a):
    return np.ascontiguousarray(np.asarray(a, np.float32).astype(ml_dtypes.bfloat16))


def _tables(NBLK, s):
    NOWN = NBLK // 2
    T = {}
    gam = 1.0 - np.exp2(-5.0 - np.arange(4, dtype=np.float64))
    p = np.arange(128)
    i = np.arange(256)
    dm = np.zeros((128, 4, 2, 256))
    for h in range(4):
        for jh in range(2):
            j = jh * 128 + p
            d = i[None, :] - j[:, None]
            dm[:, h, jh, :] = np.where(d >= 0, gam[h] ** np.maximum(d, 0), 0.0)
    T["t_dmask"] = dm.astype(np.float32)
    ke = np.zeros((128, 2, 4, 128))
    for h in range(4):
        for half in range(2):
            ke[:, half, h, :] = (gam[h] ** (255.0 - (half * 128 + p)))[:, None] * 128.0 ** -0.5
    T["t_kend"] = ke.reshape(128, 2, 512).astype(np.float32)
    qf = np.zeros((128, 4, 256))
    for h in range(4):
        qf[:, h, :] = (gam[h] ** (i + 1.0))[None, :]
    T["t_qfs"] = qf.astype(np.float32)
    T["cdecay"] = [float(gam[h] ** 256.0) for h in range(4)]
    slopes = np.exp2(-(np.arange(8) + 1.0))
    TL = NBLK * 256
    t = np.arange(TL)
    ks = np.zeros((34, TL))
    ks[t // 256, t] = 1.0
    ks[32, :] = 1.0
    ks[33, :] = t % 256
    T["t_kstat"] = _bf(ks)
    qs = np.zeros((2, 8, 256))
    for h in range(8):
        qs[0, h, :] = -slopes[h] * i
        qs[1, h, :] = slopes[h]
    T["t_qstat"] = _bf(qs)
    cb = np.zeros((128, 2, 256))
    for tt in range(2):
        cb[:, tt, :] = np.where(i[None, :] >= (tt * 128 + p)[:, None], 0.0, -BIG)
    T["t_cb"] = _bf(cb)
    T["t_ident"] = _bf(np.eye(128))
    T["t_identf"] = np.eye(128, dtype=np.float32)
    vb = np.zeros((NOWN, 32))
    mb = np.zeros((NOWN, 8, 32))
    n = np.arange(32)
    for j in range(NOWN):
        c = 2 * j + 1
        valid = (n < c) & ~((s == 0) & (n == 0))
        vb[j] = np.where(valid, 0.0, -BIG)
        vb[j, c] = -2 * BIG
        for h in range(8):
            mb[j, h] = np.where(valid, -slopes[h] * 256.0 * (c - n) - BIG, -2 * BIG)
            mb[j, h, c] = 0.0
    T["t_vb"] = np.ascontiguousarray(np.broadcast_to(vb[None, :, None, :], (128, NOWN, 4, 32))).astype(np.float32)
    T["t_mb"] = np.ascontiguousarray(np.broadcast_to(mb[None], (128, NOWN, 8, 32))).astype(np.float32)
    return T


def _core_inputs(inputs, b, s, NBLK):
    NOWN = NBLK // 2
    TL = NBLK * 256
    x = np.asarray(inputs["x"], np.float32)
    xb = x[b]
    xT = np.zeros((D, TL), np.float32)
    if s == 1:
        xT[:, :] = xb[:TL].T
    else:
        xT[:, 256:] = xb[:TL - 256].T
    own = [2 * j + s for j in range(NOWN)]
    xo = np.concatenate([xb[g * 256:(g + 1) * 256] for g in own], axis=0)
    xTt = np.ascontiguousarray(xT.reshape(8, 128, NBLK, 256).transpose(2, 1, 0, 3)).reshape(NBLK, 128, 2048)
    m = {"xT": xTt, "xo": np.ascontiguousarray(xo)}
    m["w_in"] = np.ascontiguousarray(inputs["w_in"][0], np.float32)
    m["w_out"] = np.ascontiguousarray(inputs["w_out"][0], np.float32)
    for nm in ("w_gate", "w_up"):
        w = np.asarray(inputs[nm][0], np.float32)
        m[nm] = np.ascontiguousarray(w.reshape(8, 128, 11, 256).transpose(2, 1, 0, 3)).reshape(11, 128, 2048)
    m["w_down"] = np.ascontiguousarray(inputs["w_down"][0], np.float32)
    m["gn"] = np.ascontiguousarray(np.asarray(inputs["ret_gn_gain"][0], np.float32).reshape(4, 128).T)
    lnp = np.stack([inputs["ln1_g"][0], inputs["ln1_b"][0], inputs["ln2_g"][0], inputs["ln2_b"][0]]).astype(np.float32)
    m["lnp"] = np.ascontiguousarray(np.broadcast_to(lnp[None], (128, 4, D)))
    T = _tables(NBLK, s)
    for k, v in T.items():
        if k != "cdecay":
            m[k] = v
    return m, own


def build(NBLK=32, debug=False):
    NOWN = NBLK // 2
    TL = NBLK * 256
    TO = NOWN * 256
    NG = TO // 512
    cdecay = _tables(2, 1)["cdecay"]

    nc = bass.Bass("TRN2", target_bir_lowering=False)
    P = Prog(nc)

    def din(name, shape, dt=F32):
        return nc.dram_tensor(name, list(shape), dt, kind="ExternalInput").ap()

    xT = din("xT", [NBLK, 128, 2048])
    xo = din("xo", [TO, D])
    w_in = din("w_in", [D, 3584])
    w_out = din("w_out", [D, D])
    w_gate = din("w_gate", [11, 128, 2048])
    w_up = din("w_up", [11, 128, 2048])
    w_down = din("w_down", [DFF, D])
    gn_d = din("gn", [128, 4])
    lnp_d = din("lnp", [128, 4, D])
    t_dmask = din("t_dmask", [128, 4, 2, 256])
    t_kend = din("t_kend", [128, 2, 512])
    t_qfs = din("t_qfs", [128, 4, 256])
    t_kstat = din("t_kstat", [34, TL], BF16)
    t_qstat = din("t_qstat", [2, 8, 256], BF16)
    t_cb = din("t_cb", [128, 2, 256], BF16)
    t_ident = din("t_ident", [128, 128], BF16)
    t_identf = din("t_identf", [128, 128])
    t_vb = din("t_vb", [128, NOWN, 4, 32])
    t_mb = din("t_mb", [128, NOWN, 8, 32])
    y = nc.dram_tensor("y", [TO, D], F32, kind="ExternalOutput").ap()
    if debug:
        d_mix = nc.dram_tensor("d_mix", [128, 8, TO], BF16, kind="ExternalOutput").ap()
        d_qa = nc.dram_tensor("d_qa", [98, 256], BF16, kind="ExternalOutput").ap()
        d_ka = nc.dram_tensor("d_ka", [98, TL], BF16, kind="ExternalOutput").ap()
        d_gsb = nc.dram_tensor("d_gsb", [128, 256], F32, kind="ExternalOutput").ap()
        d_m8 = nc.dram_tensor("d_m8", [128, 64], F32, kind="ExternalOutput").ap()
        d_mbt = nc.dram_tensor("d_mbt", [128, 192], BF16, kind="ExternalOutput").ap()
        d_va = nc.dram_tensor("d_va", [128, NBLK * 2 * 4 * 65], BF16, kind="ExternalOutput").ap()
        d_ksum = nc.dram_tensor("d_ksum", [64, 32], F32, kind="ExternalOutput").ap()
        d_mtok = nc.dram_tensor("d_mtok", [128, 512], BF16, kind="ExternalOutput").ap()
        d_h1 = nc.dram_tensor("d_h1", [TO, D], F32, kind="ExternalOutput").ap()
        d_u = nc.dram_tensor("d_u", [TO, D], F32, kind="ExternalOutput").ap()
        d_mv = nc.dram_tensor("d_mv", [128, 8], F32, kind="ExternalOutput").ap()
        d_rs = nc.dram_tensor("d_rs", [128, 8], F32, kind="ExternalOutput").ap()
        d_pt = nc.dram_tensor("d_pt", [3, 128, 512], BF16, kind="ExternalOutput").ap()
        d_rden = nc.dram_tensor("d_rden", [128, 4], F32, kind="ExternalOutput").ap()
    scr_g = nc.dram_tensor("scr_g", [11, 128, 2048], BF16).ap()
    scr_u = nc.dram_tensor("scr_u", [11, 128, 2048], BF16).ap()
    scr_d = nc.dram_tensor("scr_d", [11, 128, 2048], BF16).ap()
    B_scr = Buf("scr")

    w_in_v = w_in.rearrange("(k p) c -> p k c", p=128)
    w_out_v = w_out.rearrange("(k p) c -> p k c", p=128)
    w_down_v = w_down.rearrange("(f p) c -> p f c", p=128)

    top = ExitStack()
    with top:
        def sb(es, name, shape, dt):
            return es.enter_context(nc.sbuf_tensor("sb_" + name, list(shape), dt))

        pbank = [top.enter_context(nc.psum_tensor(f"pb{i}", [128, 512], F32)) for i in range(8)]
        PB = [Buf(f"pb{i}", excl=True) for i in range(8)]

        def pv(i):
            return pbank[i]

        def pv16(i):
            return pbank[i][:, :].bitcast(BF16)

        class Rot:
            def __init__(self, ids):
                self.ids = list(ids)
                self.i = 0

            def next(self):
                b = self.ids[self.i % len(self.ids)]
                self.i += 1
                return b

        def mm(out, lhsT, rhs, start, stop, reads, writes, **kw):
            P.op("pe", lambda e: e.matmul(out, lhsT, rhs, start=start, stop=stop, **kw), reads, writes)

        def tr(out, in_, ident, reads, writes):
            P.op("pe", lambda e: e.transpose(out, in_, ident), reads, writes)

        def act(out, in_, func, reads, writes, **kw):
            P.op("act", lambda e: e.activation(out, in_, func, **kw), reads, writes)

        def v_tt(eng, out, in0, in1, op, reads, writes):
            P.op(eng, lambda e: e.tensor_tensor(out, in0, in1, op), reads, writes)

        def v_ts(eng, out, in0, s1, s2, op0, reads, writes, op1=ALU.bypass):
            P.op(eng, lambda e: e.tensor_scalar(out, in0, s1, s2, op0, op1), reads, writes)

        def v_cp(eng, out, in_, reads, writes):
            P.op(eng, lambda e: e.tensor_copy(out, in_), reads, writes)

        def v_stt(out, in0, scalar, in1, op0, op1, reads, writes):
            P.op("dve", lambda e: e.scalar_tensor_tensor(out, in0, scalar, in1, op0, op1), reads, writes)

        mixM = sb(top, "mixM", [128, 4, TO], BF16)
        B_mixM = Buf("mixM")
        ident = sb(top, "ident", [128, 128], BF16)
        B_ident = Buf("ident")
        P.dma("sp", ident[:, :], t_ident, writes=[B_ident])
        xTc = [sb(top, f"xTc{i}", [128, 8, 256], BF16) for i in range(2)]
        B_xTc = [Buf(f"xTc{i}") for i in range(2)]

        def load_xT(c):
            P.dma("pool", xTc[c % 2][:, :, :].rearrange("p k t -> p (k t)"), xT[c], writes=[B_xTc[c % 2]])

        with ExitStack() as es:
            wMs = [sb(es, f"wM{i}", [128, 8, 768], BF16) for i in range(2)]
            B_wMs = [[Buf(f"wM{i}_{k}") for k in range(3)] for i in range(2)]

            def load_wM_seg(g, seg):
                d0, s0 = [(0, 2048 + 256 * g), (256, 2560 + 256 * g), (512, 3072 + 256 * g)][seg]
                P.dma("pool", wMs[g][:, :, d0:d0 + 256], w_in_v[:, :, s0:s0 + 256], writes=[B_wMs[g][seg]])

            def load_wM(g):
                for seg in (1, 2, 0):
                    load_wM_seg(g, seg)
            KA = [sb(es, f"KA{h}", [98, TL], BF16) for h in range(4)]
            B_KA = [Buf(f"KA{h}") for h in range(4)]
            VA = sb(es, "VA", [128, NBLK * 2, 4, 65], BF16)
            B_VA = Buf("VA")
            ksum = [sb(es, f"ksum{h}", [64, 32], F32) for h in range(4)]
            B_ksum = [Buf(f"ksum{h}") for h in range(4)]
            kmT = [sb(es, f"kmT{h}", [64, 32], BF16) for h in range(4)]
            B_kmT = [Buf(f"kmT{h}") for h in range(4)]
            QA = [[sb(es, f"QA{h}_{i}", [98, 256], BF16) for i in range(2)] for h in range(4)]
            B_QA = [[Buf(f"QA{h}_{i}") for i in range(2)] for h in range(4)]
            gsb = sb(es, "gsb", [128, 2, 4, 32], F32)
            m8 = sb(es, "m8", [128, 2, 4, 8], F32)
            selb = sb(es, "selb", [128, 2, 4, 32], F32)
            B_gate = [Buf("gate0"), Buf("gate1")]
            MBt = [sb(es, f"MBt{i}", [128, 192], BF16) for i in range(2)]
            B_MBt = [Buf("MBt0"), Buf("MBt1")]
            PT = [sb(es, f"PT{i}", [128, 512], BF16) for i in range(3)]
            B_PT = [Buf(f"PT{i}") for i in range(3)]
            mtok = sb(es, "mtok", [128, 2, 256], BF16)
            B_mtok = Buf("mtok")
            rden = sb(es, "rden", [128, 4], F32)
            B_rden = [Buf(f"rden{i}") for i in range(4)]
            vb = sb(es, "vb", [128, NOWN, 4, 32], F32)
            B_vb = Buf("vb")
            mbt = sb(es, "mbt", [128, NOWN, 4, 32], F32)
            B_mbt = Buf("mbt")
            cb = sb(es, "cb", [128, 2, 256], BF16)
            B_cb = Buf("cb")

            P.dma("sp", cb[:, :, :], t_cb, writes=[B_cb])
            P.dma("sp", vb[:, :, :, :], t_vb, writes=[B_vb])
            for h in range(4):
                P.dma("sp", KA[h][64:98, :], t_kstat, writes=[B_KA[h]])
            P.op("pool", lambda e: e.memset(VA[:, :, :, 64:65], 1.0), writes=[B_VA])
            for i in range(2):
                P.op("pool", lambda e, i=i: e.memset(MBt[i][:, 0:64], 0.0), writes=[B_MBt[i]])

            SB_ = Rot([0, 1, 2])
            OB_ = Rot([3, 4])
            MB_ = Rot([5, 6, 7])

            load_wM_seg(0, 1)
            load_xT(0)
            load_wM_seg(0, 2)
            load_xT(1)
            load_wM_seg(0, 0)
            for g in range(2):
                wM = wMs[g]
                B_wM = B_wMs[g]
                P.dma("sp", mbt[:, :, :, :], t_mb[:, :, 4 * g:4 * g + 4, :], writes=[B_mbt])
                for h in range(4):
                    P.op("pool", lambda e, h=h: e.memset(ksum[h][:, :], 0.0), writes=[B_ksum[h]])
                    for i in range(2):
                        P.dma("sp", QA[h][i][96:98, :], t_qstat[:, 4 * g + h, :], writes=[B_QA[h][i]])
                def proj_kv(c, g=g, wM=wM, B_wM=B_wM):
                    xi = c % 2
                    for hp in range(2):
                        bk = MB_.next()
                        for hh in range(2):
                            hl = 2 * hp + hh
                            for kt in range(8):
                                mm(pv(bk)[0:64, hh * 256:(hh + 1) * 256], wM[:, kt, 256 + hl * 64:256 + (hl + 1) * 64],
                                   xTc[xi][:, kt, :], kt == 0, kt == 7, [B_wM[1], B_xTc[xi]], [PB[bk]])
                        for hh in range(2):
                            hl = 2 * hp + hh
                            act(KA[hl][0:64, c * 256:(c + 1) * 256], pv(bk)[0:64, hh * 256:(hh + 1) * 256], AF.Copy,
                                [PB[bk]], [B_KA[hl], B_ksum[hl]], accum_out=ksum[hl][:, c:c + 1])
                    bk = MB_.next()
                    for half in range(2):
                        for kt in range(8):
                            mm(pv(bk)[:, half * 256:(half + 1) * 256], xTc[xi][:, kt, half * 128:(half + 1) * 128],
                               wM[:, kt, 512:768], kt == 0, kt == 7, [B_wM[2], B_xTc[xi]], [PB[bk]])
                    v_cp("dve", VA[:, 2 * c:2 * c + 2, :, 0:64],
                         pv(bk)[:, :].rearrange("p (t h d) -> p t h d", t=2, h=4), [PB[bk]], [B_VA])

                def proj_q_gate(c, g=g, wM=wM, B_wM=B_wM):
                    xi = c % 2
                    j = (c - 1) // 2
                    qb = j % 2
                    for hp in range(2):
                        bk = MB_.next()
                        for hh in range(2):
                            hl = 2 * hp + hh
                            for kt in range(8):
                                mm(pv(bk)[0:64, hh * 256:(hh + 1) * 256], wM[:, kt, hl * 64:(hl + 1) * 64],
                                   xTc[xi][:, kt, :], kt == 0, kt == 7, [B_wM[0], B_xTc[xi]], [PB[bk]])
                        for hh in range(2):
                            hl = 2 * hp + hh
                            v_ts("dve", QA[hl][qb][0:64, :], pv(bk)[0:64, hh * 256:(hh + 1) * 256], 0.125, None, ALU.mult,
                                 [PB[bk]], [B_QA[hl][qb]])
                    for hl in range(4):
                        v_ts("dve", kmT[hl][:, :], ksum[hl][:, :], 1.0 / 256.0, None, ALU.mult, [B_ksum[hl]], [B_kmT[hl]])
                    gk = MB_.next()
                    for qh in range(2):
                        for hl in range(4):
                            o0 = (qh * 4 + hl) * 32
                            mm(pv(gk)[:, o0:o0 + 32], QA[hl][qb][0:64, qh * 128:(qh + 1) * 128], kmT[hl][:, :],
                               True, True, [B_QA[hl][qb], B_kmT[hl]], [PB[gk]])
                    for qh in range(2):
                        v_tt("dve", gsb[:, qh, :, :], pv(gk)[:, qh * 128:(qh + 1) * 128].rearrange("p (h n) -> p h n", h=4),
                             vb[:, j, :, :], ALU.add, [PB[gk], B_vb], [B_gate[qh]])
                        for hl in range(4):
                            P.op("dve", lambda e, qh=qh, hl=hl: e.max(m8[:, qh, hl, :], gsb[:, qh, hl, :]),
                                 [], [B_gate[qh]])
                        v_tt("dve", selb[:, qh, :, :], gsb[:, qh, :, :], m8[:, qh, :, 2:3].broadcast_to([128, 4, 32]), ALU.is_ge,
                             [], [B_gate[qh]])
                        v_stt(MBt[qh][:, 64:192].rearrange("p (h n) -> p h n", h=4), selb[:, qh, :, :], BIG,
                              mbt[:, j, :, :], ALU.mult, ALU.add, [B_gate[qh], B_mbt], [B_MBt[qh]])

                def mask_T(c, g=g):
                    j = (c - 1) // 2
                    qb = j % 2
                    tk = MB_.next()
                    for hl in range(4):
                        for qh in range(2):
                            o0 = (hl * 2 + qh) * 128
                            tr(pv16(tk)[0:96, o0:o0 + 128], MBt[qh][:, 32 * hl:32 * hl + 96], ident[:, :],
                               [B_MBt[qh], B_ident], [PB[tk]])
                    for hl in range(4):
                        v_cp("dve", QA[hl][qb][64:96, :], pv16(tk)[64:96, hl * 256:(hl + 1) * 256], [PB[tk]], [B_QA[hl][qb]])

                def attention(c, hook, hook2, g=g):
                    j = (c - 1) // 2
                    qb = j % 2
                    items = [(hl, p_) for hl in range(4) for p_ in range(c + 1)]
                    info = {}
                    LOOK = 2
                    for it in range(len(items) + LOOK):
                        if it == 2 * (c + 1):
                            hook()
                        if it == 3 * (c + 1) + (c + 1) // 2:
                            hook2()
                        if it < len(items):
                            hl, p_ = items[it]
                            sbk = SB_.next()
                            pti = it % 3
                            diag = (p_ == c)
                            for t in range(2):
                                kt = 2 * p_ + t
                                mm(pv(sbk)[:, t * 256:(t + 1) * 256], KA[hl][0:98, kt * 128:(kt + 1) * 128], QA[hl][qb][0:98, :],
                                   True, not diag, [B_KA[hl], B_QA[hl][qb]], [PB[sbk]])
                                if diag:
                                    mm(pv(sbk)[:, t * 256:(t + 1) * 256], ident[:, :], cb[:, t, :], False, True,
                                       [B_ident, B_cb], [PB[sbk]])
                            act(PT[pti][:, :], pv(sbk)[:, :], AF.Exp, [PB[sbk]], [B_PT[pti]])
                            info[it] = pti
                        if it - LOOK >= 0:
                            it2 = it - LOOK
                            hl, p_ = items[it2]
                            pti = info[it2]
                            diag = (p_ == c)
                            if p_ == 0:
                                ob = OB_.next()
                                info[("ob", hl)] = ob
                            ob = info[("ob", hl)]
                            for t in range(2):
                                kt = 2 * p_ + t
                                for qh in range(2):
                                    if diag and t == 1 and qh == 0:
                                        continue
                                    first = (p_ == 0 and t == 0 and qh == 0)
                                    mm(pv(ob)[:, qh * 128:qh * 128 + 65], PT[pti][:, t * 256 + qh * 128:t * 256 + (qh + 1) * 128],
                                       VA[:, kt, hl, :], first, diag and t == 1 and qh == 1, [B_PT[pti], B_VA], [PB[ob]],
                                       skip_group_check=True)
                            if diag:
                                for qh in range(2):
                                    ri = (hl % 2) * 2 + qh
                                    P.op("dve", lambda e, ob=ob, qh=qh, ri=ri: e.reciprocal(rden[:, ri:ri + 1], pv(ob)[:, qh * 128 + 64:qh * 128 + 65]),
                                         [PB[ob]], [B_rden[ri]])
                                    v_ts("dve", mtok[:, qh, hl * 64:(hl + 1) * 64], pv(ob)[:, qh * 128:qh * 128 + 64],
                                         rden[:, ri:ri + 1], None, ALU.mult, [PB[ob], B_rden[ri]], [B_mtok])
                    tk = MB_.next()
                    for pr in range(2):
                        for qh in range(2):
                            o0 = (pr * 2 + qh) * 128
                            tr(pv16(tk)[:, o0:o0 + 128], mtok[:, qh, pr * 128:(pr + 1) * 128], ident[:, :],
                               [B_mtok, B_ident], [PB[tk]])
                    for pr in range(2):
                        v_cp("dve", mixM[:, 2 * g + pr, j * 256:(j + 1) * 256], pv16(tk)[:, pr * 256:(pr + 1) * 256],
                             [PB[tk]], [B_mixM])

                if g == 1:
                    load_xT(0)
                    load_xT(1)
                proj_kv(0)
                proj_kv(1)
                proj_q_gate(1)
                mask_T(1)
                for c in range(1, NBLK, 2):
                    if g == 0 and c == (NBLK // 2) + 1:
                        load_wM(1)
                    if c + 2 < NBLK:
                        load_xT(c + 1)
                        load_xT(c + 2)

                        def hook(c=c):
                            proj_kv(c + 1)
                            proj_kv(c + 2)
                            proj_q_gate(c + 2)

                        def hook2(c=c):
                            mask_T(c + 2)
                    else:
                        def hook():
                            pass

                        def hook2():
                            pass
                    attention(c, hook, hook2)
            if debug:
                qb_last = ((NBLK - 2) // 2) % 2
                P.dma("sp", d_qa, QA[0][qb_last][:, :], reads=[B_QA[0][qb_last]])
                P.dma("sp", d_ka, KA[0][:, :], reads=[B_KA[0]])
                P.dma("sp", d_gsb, gsb[:, :, :, :].rearrange("p a h n -> p (a h n)"), reads=[B_gate[0], B_gate[1]])
                P.dma("sp", d_m8, m8[:, :, :, :].rearrange("p a h n -> p (a h n)"), reads=[B_gate[0], B_gate[1]])
                P.dma("sp", d_mbt, MBt[0][:, :], reads=[B_MBt[0]])
                P.dma("sp", d_va, VA[:, :, :, :].rearrange("p a h n -> p (a h n)"), reads=[B_VA])
                P.dma("sp", d_ksum, ksum[0][:, :], reads=[B_ksum[0]])
                P.dma("sp", d_mtok, mtok[:, :, :].rearrange("p a n -> p (a n)"), reads=[B_mtok])
                for i in range(3):
                    P.dma("sp", d_pt[i], PT[i][:, :], reads=[B_PT[i]])
                P.dma("sp", d_rden, rden[:, :], reads=B_rden)
            P.barrier()

        with ExitStack() as es2:
            mixR = sb(es2, "mixR", [128, 4, TO], BF16)
            B_mixR = Buf("mixR")

            with ExitStack() as es:
                wR = sb(es, "wR", [128, 8, 2048], BF16)
                B_wRs = [Buf(f"wR{k}") for k in range(4)]
                dmask = sb(es, "dmask", [128, 4, 2, 256], F32)
                kend = sb(es, "kend", [128, 2, 512], F32)
                qfs = sb(es, "qfs", [128, 4, 256], F32)
                gn = sb(es, "gn", [128, 4], F32)
                B_tab = Buf("tabC")
                onesd = sb(es, "onesd", [128, 128], F32)
                B_ones = Buf("onesd")
                stg = [sb(es, f"stg{i}", [128, 2048], BF16) for i in range(2)]
                B_stg = [Buf(f"stg{i}") for i in range(2)]
                B_stgo = [Buf(f"stgo{i}") for i in range(2)]
                kT = [sb(es, f"kT{i}", [128, 4, 256], BF16) for i in range(2)]
                B_kT = [Buf(f"kT{i}") for i in range(2)]
                ktok = [sb(es, f"ktok{i}", [128, 2, 512], BF16) for i in range(2)]
                B_ktok = [Buf(f"ktok{i}") for i in range(2)]
                vtok = [sb(es, f"vtok{i}", [128, 2, 512], BF16) for i in range(2)]
                B_vtok = [Buf(f"vtok{i}") for i in range(2)]
                state = sb(es, "state", [128, 4, 128], F32)
                B_state = Buf("state")
                stbf = [sb(es, f"stbf{i}", [128, 4, 128], BF16) for i in range(2)]
                B_stbf = [Buf(f"stbf{i}") for i in range(2)]
                qT = [sb(es, f"qT{i}", [128, 4, 256], BF16) for i in range(2)]
                B_qT = [Buf(f"qT{i}") for i in range(2)]
                qsT = [sb(es, f"qsT{i}", [128, 4, 256], BF16) for i in range(2)]
                B_qsT = [Buf(f"qsT{i}") for i in range(2)]
                gsil = [sb(es, f"gsil{i}", [128, 4, 256], F32) for i in range(2)]
                B_gsil = [Buf(f"gsil{i}") for i in range(2)]
                sT = [sb(es, f"sT{i}", [128, 4, 2, 256], BF16) for i in range(2)]
                B_sT = [Buf(f"sT{i}") for i in range(2)]
                gnt = [[sb(es, f"gnt{i}_{k}", [128, 256], F32) for k in range(6)] for i in range(4)]
                B_gnt = [[Buf(f"gnt{i}_{k}") for k in range(6)] for i in range(4)]

                P.dma("sp", dmask[:, :, :, :], t_dmask, writes=[B_tab])
                P.dma("sp", kend[:, :, :], t_kend, writes=[B_tab])
                P.dma("sp", qfs[:, :, :], t_qfs, writes=[B_tab])
                P.dma("sp", gn[:, :], gn_d, writes=[B_tab])
                P.op("pool", lambda e: e.memset(onesd[:, :], 1.0 / 128.0), writes=[B_ones])
                P.op("pool", lambda e: e.memset(state[:, :, :], 0.0), writes=[B_state])
                for q4 in (1, 2):
                    P.dma("pool", wR[:, :, q4 * 512:(q4 + 1) * 512], w_in_v[:, :, q4 * 512:(q4 + 1) * 512], writes=[B_wRs[q4]])
                load_xT(0)
                for q4 in (0, 3):
                    P.dma("pool", wR[:, :, q4 * 512:(q4 + 1) * 512], w_in_v[:, :, q4 * 512:(q4 + 1) * 512], writes=[B_wRs[q4]])

                precast = []
                for i in range(11):
                    precast.append((scr_g[i], w_gate[i], "pkc"))
                    precast.append((scr_u[i], w_up[i], "pkc"))
                    precast.append((scr_d[i], w_down_v[:, 2 * i:2 * i + 2, :], "pfc"))
                pc_i = [0]

                def do_precast(n):
                    for _ in range(n):
                        if pc_i[0] >= len(precast):
                            return
                        dst, src, kind = precast[pc_i[0]]
                        si = pc_i[0] % 2
                        pc_i[0] += 1
                        if kind == "pkc":
                            P.dma("pool", stg[si][:, :], src, writes=[B_stg[si]])
                        else:
                            P.dma("pool", stg[si][:, :].rearrange("p (f c) -> p f c", f=2), src, writes=[B_stg[si]])
                        P.dma("sp", dst, stg[si][:, :], reads=[B_stg[si]], writes=[Buf("scrchunk")], semb=B_stgo[si])

                RB = Rot(list(range(8)))
                pending = [None]
                pendA = [None]
                for c in range(NBLK):
                    xi = c % 2
                    bi = c % 2
                    if c + 1 < NBLK:
                        load_xT(c + 1)
                    do_precast(2 if c < 8 else 1)
                    own = (c % 2 == 1)
                    j = (c - 1) // 2
                    oi = j % 2
                    if own:
                        for hp in range(2):
                            bk = RB.next()
                            for hh in range(2):
                                h = 2 * hp + hh
                                for kt in range(8):
                                    mm(pv(bk)[:, hh * 256:(hh + 1) * 256], wR[:, kt, 512 + h * 128:512 + (h + 1) * 128],
                                       xTc[xi][:, kt, :], kt == 0, kt == 7, [B_wRs[1], B_xTc[xi]], [PB[bk]])
                            act(kT[bi][:, 2 * hp:2 * hp + 2, :], pv(bk)[:, :].rearrange("p (h t) -> p h t", h=2), AF.Copy,
                                [PB[bk]], [B_kT[bi]], scale=128.0 ** -0.5)
                        for hp in range(2):
                            bk = RB.next()
                            for hh in range(2):
                                h = 2 * hp + hh
                                for kt in range(8):
                                    mm(pv(bk)[:, hh * 256:(hh + 1) * 256], wR[:, kt, h * 128:(h + 1) * 128],
                                       xTc[xi][:, kt, :], kt == 0, kt == 7, [B_wRs[0], B_xTc[xi]], [PB[bk]])
                            v_cp("dve", qT[oi][:, 2 * hp:2 * hp + 2, :], pv(bk)[:, :].rearrange("p (h t) -> p h t", h=2),
                                 [PB[bk]], [B_qT[oi]])
                            v_tt("dve", qsT[oi][:, 2 * hp:2 * hp + 2, :], pv(bk)[:, :].rearrange("p (h t) -> p h t", h=2),
                                 qfs[:, 2 * hp:2 * hp + 2, :], ALU.mult, [PB[bk], B_tab], [B_qsT[oi]])
                    for half in range(2):
                        bk = RB.next()
                        for kt in range(8):
                            mm(pv(bk)[:, :], xTc[xi][:, kt, half * 128:(half + 1) * 128], wR[:, kt, 512:1024],
                               kt == 0, kt == 7, [B_wRs[1], B_xTc[xi]], [PB[bk]])
                        v_tt("dve", ktok[bi][:, half, :], pv(bk)[:, :], kend[:, half, :], ALU.mult,
                             [PB[bk], B_tab], [B_ktok[bi]])
                    for half in range(2):
                        bk = RB.next()
                        for kt in range(8):
                            mm(pv(bk)[:, :], xTc[xi][:, kt, half * 128:(half + 1) * 128], wR[:, kt, 1024:1536],
                               kt == 0, kt == 7, [B_wRs[2], B_xTc[xi]], [PB[bk]])
                        act(vtok[bi][:, half, :], pv(bk)[:, :], AF.Copy, [PB[bk]], [B_vtok[bi]])
                    if pending[0] is not None:
                        pending[0]()
                        pending[0] = None
                    if pendA[0] is not None:
                        pendA[0]()
                        pendA[0] = None
                    if own:
                        for hp in range(2):
                            bk = RB.next()
                            for hh in range(2):
                                h = 2 * hp + hh
                                for kt in range(8):
                                    mm(pv(bk)[:, hh * 256:(hh + 1) * 256], wR[:, kt, 1536 + h * 128:1536 + (h + 1) * 128],
                                       xTc[xi][:, kt, :], kt == 0, kt == 7, [B_wRs[3], B_xTc[xi]], [PB[bk]])
                            act(gsil[oi][:, 2 * hp:2 * hp + 2, :], pv(bk)[:, :].rearrange("p (h t) -> p h t", h=2), AF.Silu,
                                [PB[bk]], [B_gsil[oi]])
                        v_cp("pool", stbf[oi][:, :, :], state[:, :, :], [B_state], [B_stbf[oi]])
                    bk = RB.next()
                    for h in range(4):
                        for half in range(2):
                            mm(pv(bk)[:, h * 128:(h + 1) * 128], ktok[bi][:, half, h * 128:(h + 1) * 128],
                               vtok[bi][:, half, h * 128:(h + 1) * 128], half == 0, half == 1,
                               [B_ktok[bi], B_vtok[bi]], [PB[bk]])
                    for h in range(4):
                        v_stt(state[:, h, :], state[:, h, :], cdecay[h], pv(bk)[:, h * 128:(h + 1) * 128], ALU.mult, ALU.add,
                              [PB[bk]], [B_state])
                    if not own:
                        continue
                    def gn_chain(j=j, oi=oi):
                        for h in range(4):
                            act(gnt[h][4][:, :], gnt[h][3][:, :], AF.Sqrt, [B_gnt[h][3]], [B_gnt[h][4]])
                        for h in range(4):
                            P.op("dve", lambda e, h=h: e.reciprocal(gnt[h][4][:, :], gnt[h][4][:, :]), [], [B_gnt[h][4]])
                        for h in range(4):
                            v_tt("pool", gnt[h][5][:, :], gnt[h][5][:, :], gnt[h][4][:, :], ALU.mult, [B_gnt[h][4]], [B_gnt[h][5]])
                        for h in range(4):
                            v_stt(mixR[:, h, j * 256:(j + 1) * 256], gnt[h][5][:, :], gn[:, h:h + 1], gsil[oi][:, h, :], ALU.mult, ALU.mult,
                                  [B_gnt[h][5], B_tab, B_gsil[oi]], [B_mixR])

                    def part_a(j=j, oi=oi, bi=bi, chain=gn_chain):
                        for h in range(4):
                            bk = RB.next()
                            for jh in range(2):
                                mm(pv(bk)[:, jh * 256:(jh + 1) * 256], kT[bi][:, h, jh * 128:(jh + 1) * 128], qT[oi][:, h, :],
                                   True, True, [B_kT[bi], B_qT[oi]], [PB[bk]])
                            v_tt("dve", sT[oi][:, h, :, :], pv(bk)[:, :].rearrange("p (a t) -> p a t", a=2), dmask[:, h, :, :],
                                 ALU.mult, [PB[bk], B_tab], [B_sT[oi]])
                        rbk = [RB.next(), RB.next()]
                        for h in range(4):
                            bk = rbk[h // 2]
                            o0 = (h % 2) * 256
                            for jh in range(2):
                                mm(pv(bk)[:, o0:o0 + 256], vtok[bi][:, jh, h * 128:(h + 1) * 128], sT[oi][:, h, jh, :], jh == 0, False,
                                   [B_vtok[bi], B_sT[oi]], [PB[bk]])
                            mm(pv(bk)[:, o0:o0 + 256], stbf[oi][:, h, :], qsT[oi][:, h, :], False, True, [B_stbf[oi], B_qsT[oi]], [PB[bk]])
                        for h in range(4):
                            bk = rbk[h // 2]
                            o0 = (h % 2) * 256
                            act(gnt[h][0][:, :], pv(bk)[:, o0:o0 + 256], AF.Copy, [PB[bk]], [B_gnt[h][0]])
                            act(gnt[h][1][:, :], pv(bk)[:, o0:o0 + 256], AF.Square, [PB[bk]], [B_gnt[h][1]])
                        sbs = []
                        for h in range(4):
                            bs = RB.next()
                            sbs.append(bs)
                            mm(pv(bs)[:, 0:256], onesd[:, :], gnt[h][0][:, :], True, True, [B_ones, B_gnt[h][0]], [PB[bs]])
                            mm(pv(bs)[:, 256:512], onesd[:, :], gnt[h][1][:, :], True, True, [B_ones, B_gnt[h][1]], [PB[bs]])
                        for h in range(4):
                            act(gnt[h][2][:, :], pv(sbs[h])[:, 0:256], AF.Square, [PB[sbs[h]]], [B_gnt[h][2]])
                        for h in range(4):
                            v_stt(gnt[h][3][:, :], pv(sbs[h])[:, 256:512], EPS, gnt[h][2][:, :], ALU.add, ALU.subtract,
                                  [PB[sbs[h]], B_gnt[h][2]], [B_gnt[h][3]])
                            v_tt("dve", gnt[h][5][:, :], gnt[h][0][:, :], pv(sbs[h])[:, 0:256], ALU.subtract,
                                 [B_gnt[h][0], PB[sbs[h]]], [B_gnt[h][5]])
                        pending[0] = chain

                    pendA[0] = part_a
                if pendA[0] is not None:
                    pendA[0]()
                    pendA[0] = None
                if pending[0] is not None:
                    pending[0]()
                    pending[0] = None
                do_precast(100)
                P.barrier()

            if debug:
                P.dma("sp", d_mix[:, 0:4, :], mixR[:, :, :], reads=[B_mixR])
                P.dma("sp", d_mix[:, 4:8, :], mixM[:, :, :], reads=[B_mixM])

            with ExitStack() as es:
                wo = sb(es, "wo", [128, 8, D], BF16)
                B_wo = Buf("wo")
                lnp = sb(es, "lnp", [128, 4, D], F32)
                B_lnp = Buf("lnp")
                identf = sb(es, "identf", [128, 128], F32)
                B_identf = Buf("identf")
                wg = [sb(es, f"wg{i}", [128, 8, 256], BF16) for i in range(2)]
                wu = [sb(es, f"wu{i}", [128, 8, 256], BF16) for i in range(2)]
                B_wg = [Buf(f"wg{i}") for i in range(2)]
                B_wu = [Buf(f"wu{i}") for i in range(2)]
                NWD = 4
                wd = [sb(es, f"wd{i}", [128, 2, 512], BF16) for i in range(NWD)]
                B_wd = [Buf(f"wd{i}") for i in range(NWD)]
                h1 = [sb(es, f"h1_{i}", [128, 4, D], F32) for i in range(2)]
                B_h1 = [[Buf(f"h1_{i}_{t}") for t in range(4)] for i in range(2)]
                h1T = sb(es, "h1T", [128, 8, 512], BF16)
                B_h1T = Buf("h1T")
                hidT = sb(es, "hidT", [128, NFT, 512], BF16)
                B_hidT = Buf("hidT")
                xot = [sb(es, f"xot{i}", [128, D], F32) for i in range(2)]
                B_xot = [Buf(f"xot{i}") for i in range(2)]
                gs = [sb(es, f"gs{i}", [128, 512], BF16) for i in range(2)]
                B_gs = [Buf(f"gs{i}") for i in range(2)]
                st6 = sb(es, "st6", [128, 8, 2, 6], F32)
                mv = sb(es, "mv", [128, 8, 2], F32)
                rs = sb(es, "rs", [128, 8, 2], F32)
                B_st = [Buf(f"st{i}") for i in range(8)]

                for k2 in range(4):
                    P.dma("pool", wo[:, 2 * k2:2 * k2 + 2, :], w_out_v[:, 2 * k2:2 * k2 + 2, :], writes=[B_wo])
                P.dma("sp", lnp[:, :, :], lnp_d, writes=[B_lnp])
                P.dma("sp", identf[:, :], t_identf, writes=[B_identf])
                DB = Rot(list(range(8)))
                xo_i = [0]
                scr_d_v = [scr_d[i].rearrange("p (f c) -> p f c", f=2) for i in range(11)]

                def layer_norm4(hb, gi):
                    for tt in range(4):
                        si = hb * 4 + tt
                        for a_ in range(2):
                            P.op("dve", lambda e, a_=a_, si=si, tt=tt: e.bn_stats(st6[:, si, a_, :], h1[hb][:, tt, a_ * 512:(a_ + 1) * 512]),
                                 [B_h1[hb][tt]], [B_st[si]])
                        P.op("dve", lambda e, si=si: e.bn_aggr(mv[:, si, :], st6[:, si, :, :].rearrange("p a b -> p (a b)")), [], [B_st[si]])
                        v_ts("dve", rs[:, si, 0:1], mv[:, si, 1:2], EPS, None, ALU.add, [], [B_st[si]])
                    for tt in range(4):
                        si = hb * 4 + tt
                        act(rs[:, si, 0:1], rs[:, si, 0:1], AF.Sqrt, [], [B_st[si]])
                    for tt in range(4):
                        si = hb * 4 + tt
                        P.op("dve", lambda e, si=si: e.reciprocal(rs[:, si, 1:2], rs[:, si, 0:1]), [], [B_st[si]])
                    for tt in range(4):
                        si = hb * 4 + tt
                        u = h1[hb][:, tt, :]
                        v_ts("dve", u, u, mv[:, si, 0:1], rs[:, si, 1:2], ALU.subtract, [B_st[si]], [B_h1[hb][tt]], op1=ALU.mult)
                    for tt in range(4):
                        u = h1[hb][:, tt, :]
                        v_tt("pool", u, u, lnp[:, gi, :], ALU.mult, [B_lnp], [B_h1[hb][tt]])
                        v_tt("pool", u, u, lnp[:, gi + 1, :], ALU.add, [B_lnp], [B_h1[hb][tt]])

                def layer_norm1(hb, tt, gi):
                    si = hb * 4 + tt
                    u = h1[hb][:, tt, :]
                    Bh = B_h1[hb][tt]
                    for a_ in range(2):
                        P.op("dve", lambda e, a_=a_: e.bn_stats(st6[:, si, a_, :], h1[hb][:, tt, a_ * 512:(a_ + 1) * 512]), [Bh], [B_st[si]])
                    P.op("dve", lambda e: e.bn_aggr(mv[:, si, :], st6[:, si, :, :].rearrange("p a b -> p (a b)")), [], [B_st[si]])
                    v_ts("dve", rs[:, si, 0:1], mv[:, si, 1:2], EPS, None, ALU.add, [], [B_st[si]])
                    act(rs[:, si, 0:1], rs[:, si, 0:1], AF.Sqrt, [], [B_st[si]])
                    P.op("dve", lambda e: e.reciprocal(rs[:, si, 1:2], rs[:, si, 0:1]), [], [B_st[si]])
                    v_ts("dve", u, u, mv[:, si, 0:1], rs[:, si, 1:2], ALU.subtract, [B_st[si]], [Bh], op1=ALU.mult)
                    v_tt("pool", u, u, lnp[:, gi, :], ALU.mult, [B_lnp], [Bh])
                    v_tt("pool", u, u, lnp[:, gi + 1, :], ALU.add, [B_lnp], [Bh])

                def load_gu(fc):
                    wi = fc % 2
                    P.dma("sp", wg[wi][:, :, :].rearrange("p k c -> p (k c)"), scr_g[fc], writes=[B_wg[wi]])
                    P.dma("sp", wu[wi][:, :, :].rearrange("p k c -> p (k c)"), scr_u[fc], writes=[B_wu[wi]])

                def load_wd(i):
                    half, fc = divmod(i, 11)
                    P.dma("sp", wd[i % NWD][:, :, :], scr_d_v[fc][:, :, half * 512:(half + 1) * 512], writes=[B_wd[i % NWD]])

                def stage1(G):
                    hb = G % 2
                    for tt in range(4):
                        tok0 = G * 512 + tt * 128
                        xb_ = xo_i[0] % 2
                        xo_i[0] += 1
                        P.dma("sp", xot[xb_][:, :], xo[tok0:tok0 + 128, :], writes=[B_xot[xb_]])
                        for half in range(2):
                            bk = DB.next()
                            for kt in range(8):
                                src = mixR if kt < 4 else mixM
                                Bsrc = B_mixR if kt < 4 else B_mixM
                                mm(pv(bk)[:, :], src[:, kt % 4, tok0:tok0 + 128], wo[:, kt, half * 512:(half + 1) * 512],
                                   kt == 0, kt == 7, [Bsrc, B_wo], [PB[bk]])
                            v_stt(h1[hb][:, tt, half * 512:(half + 1) * 512], xot[xb_][:, half * 512:(half + 1) * 512], ALPHA,
                                  pv(bk)[:, :], ALU.mult, ALU.add, [B_xot[xb_], PB[bk]], [B_h1[hb][tt]])
                    layer_norm4(hb, 0)
                    if debug:
                        for tt in range(4):
                            tok0 = G * 512 + tt * 128
                            P.dma("sp", d_h1[tok0:tok0 + 128, :], h1[hb][:, tt, :], reads=[B_h1[hb][tt]])

                def stage2a(G):
                    hb = G % 2
                    for tt in range(4):
                        for k4 in range(2):
                            bk = DB.next()
                            for kk in range(4):
                                kt = k4 * 4 + kk
                                tr(pv(bk)[:, kk * 128:(kk + 1) * 128], h1[hb][:, tt, kt * 128:(kt + 1) * 128], identf[:, :],
                                   [B_h1[hb][tt], B_identf], [PB[bk]])
                            act(h1T[:, k4 * 4:k4 * 4 + 4, tt * 128:(tt + 1) * 128], pv(bk)[:, :].rearrange("p (k t) -> p k t", k=4),
                                AF.Copy, [PB[bk]], [B_h1T])

                def stage2b(G, ln2_of=None):
                    for fc in range(11):
                        wi = fc % 2
                        if ln2_of is not None and fc in (2, 4, 6, 8):
                            stage3b_tile(ln2_of, (fc - 2) // 2)
                        for fi in range(2):
                            ft = fc * 2 + fi
                            bg = DB.next()
                            bu = DB.next()
                            for kt in range(8):
                                mm(pv(bg)[:, :], wg[wi][:, kt, fi * 128:(fi + 1) * 128], h1T[:, kt, :], kt == 0, kt == 7,
                                   [B_wg[wi], B_h1T], [PB[bg]])
                            for kt in range(8):
                                mm(pv(bu)[:, :], wu[wi][:, kt, fi * 128:(fi + 1) * 128], h1T[:, kt, :], kt == 0, kt == 7,
                                   [B_wu[wi], B_h1T], [PB[bu]])
                            gi_ = ft % 2
                            act(gs[gi_][:, :], pv(bg)[:, :], AF.Silu, [PB[bg]], [B_gs[gi_]])
                            v_tt("dve", hidT[:, ft, :], gs[gi_][:, :], pv(bu)[:, :], ALU.mult, [B_gs[gi_], PB[bu]], [B_hidT])
                        if fc + 2 < 11:
                            load_gu(fc + 2)
                    for i in range(NWD):
                        load_wd(i)
                    if G + 1 < NG:
                        load_gu(0)
                        load_gu(1)

                def stage3a(G):
                    hb = G % 2
                    for half in range(2):
                        banks = [DB.next() for _ in range(4)]
                        for fc in range(11):
                            i = half * 11 + fc
                            for fi in range(2):
                                ft = fc * 2 + fi
                                for tt in range(4):
                                    mm(pv(banks[tt])[:, :], hidT[:, ft, tt * 128:(tt + 1) * 128], wd[i % NWD][:, fi, :],
                                       ft == 0, ft == NFT - 1, [B_hidT, B_wd[i % NWD]], [PB[banks[tt]]])
                            if i + NWD < 22:
                                load_wd(i + NWD)
                        for tt in range(4):
                            v_stt(h1[hb][:, tt, half * 512:(half + 1) * 512], h1[hb][:, tt, half * 512:(half + 1) * 512], ALPHA,
                                  pv(banks[tt])[:, :], ALU.mult, ALU.add, [PB[banks[tt]]], [B_h1[hb][tt]])

                def stage3b_tile(G, tt):
                    hb = G % 2
                    tok0 = G * 512 + tt * 128
                    layer_norm1(hb, tt, 2)
                    P.dma("sp", y[tok0:tok0 + 128, :], h1[hb][:, tt, :], reads=[B_h1[hb][tt]])

                def stage3b(G):
                    hb = G % 2
                    layer_norm4(hb, 2)
                    for tt in range(4):
                        tok0 = G * 512 + tt * 128
                        P.dma("sp", y[tok0:tok0 + 128, :], h1[hb][:, tt, :], reads=[B_h1[hb][tt]])

                load_gu(0)
                load_gu(1)
                stage1(0)
                stage2a(0)
                stage2b(0)
                for G in range(NG):
                    if G + 1 < NG:
                        stage1(G + 1)
                    stage3a(G)
                    if G + 1 < NG:
                        stage2a(G + 1)
                        stage2b(G + 1, ln2_of=G)
                    else:
                        stage3b(G)
                P.barrier()

    P.emit()
    return nc


_CACHE = {}


def run(inputs, NBLK=32, debug=False, cores=None, trace=False):
    B = np.asarray(inputs["x"]).shape[0]
    key = (NBLK, debug)
    if key not in _CACHE:
        _CACHE[key] = build(NBLK, debug)
    nc = _CACHE[key]
    plan = [(b, s) for b in range(B) for s in range(2)]
    if cores is not None:
        plan = plan[:cores]
    in_maps, owns = [], []
    for (b, s) in plan:
        m, own = _core_inputs(inputs, b, s, NBLK)
        in_maps.append(m)
        owns.append(own)
    res = run_bass_kernel_spmd(nc, in_maps, core_ids=list(range(len(plan))), **({"trace": True} if trace else {}))
    S = NBLK * 256
    out = np.zeros((B, S, D), np.float32)
    dbg = {}
    for ci, (b, s) in enumerate(plan):
        yo = np.asarray(res.results[ci]["y"], np.float32)
        for j, g in enumerate(owns[ci]):
            out[b, g * 256:(g + 1) * 256] = yo[j * 256:(j + 1) * 256]
        if debug:
            dbg[(b, s)] = (np.asarray(res.results[ci]["d_mix"]), owns[ci], {k: np.asarray(v) for k, v in res.results[ci].items()})
    return out, dbg, res


def kernel(x, w_in, ret_gn_gain, w_out, ln1_g, ln1_b, w_gate, w_up, w_down, ln2_g, ln2_b):
    inputs = dict(x=x, w_in=w_in, ret_gn_gain=ret_gn_gain, w_out=w_out, ln1_g=ln1_g, ln1_b=ln1_b,
                  w_gate=w_gate, w_up=w_up, w_down=w_down, ln2_g=ln2_g, ln2_b=ln2_b)
    out, _, _ = run(inputs, NBLK=32)
    return out
```

```python
import numpy as np
import ml_dtypes
from contextlib import ExitStack
import concourse.bass as bass
import concourse.mybir as mybir
from concourse.bass_utils import run_bass_kernel_spmd

F32 = mybir.dt.float32
BF16 = mybir.dt.bfloat16
AF = mybir.ActivationFunctionType
ALU = mybir.AluOpType

D = 1024
DFF = 2816
NFT = DFF // 128
BIG = 30000.0
ALPHA = 2.0 ** 0.25
EPS = 1e-5
ENG = ("pe", "act", "dve", "pool", "sp")
SAME_ENGINE_SYNC = False


class Sem:
    def __init__(self):
        self.h = None
        self.count = 0


class Buf:
    def __init__(self, name, excl=False):
        self.name = name
        self.excl = excl
        self.w = {}
        self.r = {}
        self.dsem = None


class Prog:
    def __init__(self, nc):
        self.nc = nc
        self.ops = []
        self.q = {e: [] for e in ENG}
        self.dsems = []

    def _deps(self, eng, reads, writes, own=None):
        deps = {}

        def add(k, v, is_write):
            if isinstance(k, tuple):
                if k[1] is own:
                    return
                deps[k] = max(deps.get(k, 0), v)
            else:
                if k == eng and (eng == "pe" or not is_write):
                    return
                deps[k] = max(deps.get(k, -1), v)

        for b in reads:
            for k, v in b.w.items():
                add(k, v, True)
        for b in writes:
            for k, v in b.w.items():
                add(k, v, True)
            for k, v in b.r.items():
                add(k, v, False)
        return deps

    def _push(self, eng, fn, deps, dma=None):
        for k, v in deps.items():
            if not isinstance(k, tuple):
                self.ops[v]["signal"] = True
        idx = len(self.ops)
        self.ops.append(dict(eng=eng, fn=fn, deps=deps, signal=False, dma=dma, sid=None))
        self.q[eng].append(idx)
        return idx

    def op(self, eng, fn, reads=(), writes=()):
        ex = [b for b in reads if b.excl]
        reads = [b for b in reads if not b.excl]
        writes = list(writes) + ex
        deps = self._deps(eng, reads, writes)
        idx = self._push(eng, fn, deps)
        for b in reads:
            b.r[eng] = idx
        for b in writes:
            b.w[eng] = idx
        return idx

    def dma(self, queue, out_ap, in_ap, reads=(), writes=(), semb=None):
        b0 = semb or (writes[0] if writes else reads[0])
        if b0.dsem is None:
            b0.dsem = Sem()
            self.dsems.append(b0.dsem)
        s = b0.dsem
        deps = self._deps(None, list(reads), list(writes), own=s)
        s.count += 16
        val = s.count
        self._push(queue, lambda e: e.dma_start(out=out_ap, in_=in_ap), deps, dma=(s, val))
        for b in reads:
            b.r[("dma", s)] = val
        for b in writes:
            b.w[("dma", s)] = val

    def barrier(self):
        last = {}
        for e in ENG:
            for idx in reversed(self.q[e]):
                o = self.ops[idx]
                if o["fn"] is not None and o["dma"] is None:
                    last[e] = idx
                    break
        for e in ENG:
            deps = {}
            for k, idx in last.items():
                if k != e:
                    deps[k] = idx
            for s in self.dsems:
                if s.count:
                    deps[("dma", s)] = s.count
            self._push(e, None, deps)

    def emit(self):
        nc = self.nc
        SEG = 20000
        cnt = {e: 0 for e in ENG}
        for o in self.ops:
            if o["signal"]:
                cnt[o["eng"]] += 1
                o["sid"] = cnt[o["eng"]]
        with ExitStack() as es:
            esem = {}
            for e in ENG:
                esem[e] = [es.enter_context(nc.semaphore(f"s_{e}_{i}")) for i in range(cnt[e] // SEG + 1)]
            for i, s in enumerate(self.dsems):
                s.h = es.enter_context(nc.semaphore(f"d{i}"))
            block = es.enter_context(nc.Block())

            def run(name):
                def f(e):
                    waited = {}
                    for idx in self.q[name]:
                        o = self.ops[idx]
                        for k, v in o["deps"].items():
                            if isinstance(k, tuple):
                                sem, val, key = k[1].h, v, ("d", id(k[1]))
                            else:
                                sid = self.ops[v]["sid"]
                                seg = (sid - 1) // SEG
                                sem, val, key = esem[k][seg], sid - seg * SEG, (k, seg)
                            if waited.get(key, 0) >= val:
                                continue
                            waited[key] = val
                            e.wait_ge(sem, val)
                        if o["fn"] is None:
                            continue
                        ins = o["fn"](e)
                        if o["dma"] is not None:
                            ins.then_inc(o["dma"][0].h, 16)
                        elif o["signal"]:
                            seg = (o["sid"] - 1) // SEG
                            ins.then_inc(esem[name][seg], 1)
                return f

            block.tensor(run("pe"))
            block.scalar(run("act"))
            block.vector(run("dve"))
            block.gpsimd(run("pool"))
            block.sync(run("sp"))


def _bf(a):
    return np.ascontiguousarray(np.asarray(a, np.float32).astype(ml_dtypes.bfloat16))


def _tables(NBLK, s):
    NOWN = NBLK // 2
    T = {}
    gam = 1.0 - np.exp2(-5.0 - np.arange(4, dtype=np.float64))
    p = np.arange(128)
    i = np.arange(256)
    dm = np.zeros((128, 4, 2, 256))
    for h in range(4):
        for jh in range(2):
            j = jh * 128 + p
            d = i[None, :] - j[:, None]
            dm[:, h, jh, :] = np.where(d >= 0, gam[h] ** np.maximum(d, 0), 0.0)
    T["t_dmask"] = dm.astype(np.float32)
    ke = np.zeros((128, 2, 4, 128))
    for h in range(4):
        for half in range(2):
            ke[:, half, h, :] = (gam[h] ** (255.0 - (half * 128 + p)))[:, None] * 128.0 ** -0.5
    T["t_kend"] = ke.reshape(128, 2, 512).astype(np.float32)
    qf = np.zeros((128, 4, 256))
    for h in range(4):
        qf[:, h, :] = (gam[h] ** (i + 1.0))[None, :]
    T["t_qfs"] = qf.astype(np.float32)
    T["cdecay"] = [float(gam[h] ** 256.0) for h in range(4)]
    slopes = np.exp2(-(np.arange(8) + 1.0))
    TL = NBLK * 256
    t = np.arange(TL)
    ks = np.zeros((34, TL))
    ks[t // 256, t] = 1.0
    ks[32, :] = 1.0
    ks[33, :] = t % 256
    T["t_kstat"] = _bf(ks)
    qs = np.zeros((2, 8, 256))
    for h in range(8):
        qs[0, h, :] = -slopes[h] * i
        qs[1, h, :] = slopes[h]
    T["t_qstat"] = _bf(qs)
    cb = np.zeros((128, 2, 256))
    for tt in range(2):
        cb[:, tt, :] = np.where(i[None, :] >= (tt * 128 + p)[:, None], 0.0, -BIG)
    T["t_cb"] = _bf(cb)
    T["t_ident"] = _bf(np.eye(128))
    T["t_identf"] = np.eye(128, dtype=np.float32)
    vb = np.zeros((NOWN, 32))
    mb = np.zeros((NOWN, 8, 32))
    n = np.arange(32)
    for j in range(NOWN):
        c = 2 * j + 1
        valid = (n < c) & ~((s == 0) & (n == 0))
        vb[j] = np.where(valid, 0.0, -BIG)
        vb[j, c] = -2 * BIG
        for h in range(8):
            mb[j, h] = np.where(valid, -slopes[h] * 256.0 * (c - n) - BIG, -2 * BIG)
            mb[j, h, c] = 0.0
    T["t_vb"] = np.ascontiguousarray(np.broadcast_to(vb[None, :, None, :], (128, NOWN, 4, 32))).astype(np.float32)
    T["t_mb"] = np.ascontiguousarray(np.broadcast_to(mb[None], (128, NOWN, 8, 32))).astype(np.float32)
    return T


def _core_inputs(inputs, b, s, NBLK):
    NOWN = NBLK // 2
    TL = NBLK * 256
    x = np.asarray(inputs["x"], np.float32)
    xb = x[b]
    xT = np.zeros((D, TL), np.float32)
    if s == 1:
        xT[:, :] = xb[:TL].T
    else:
        xT[:, 256:] = xb[:TL - 256].T
    own = [2 * j + s for j in range(NOWN)]
    xo = np.concatenate([xb[g * 256:(g + 1) * 256] for g in own], axis=0)
    xTt = np.ascontiguousarray(xT.reshape(8, 128, NBLK, 256).transpose(2, 1, 0, 3)).reshape(NBLK, 128, 2048)
    m = {"xT": xTt, "xo": np.ascontiguousarray(xo)}
    m["w_in"] = np.ascontiguousarray(inputs["w_in"][0], np.float32)
    m["w_out"] = np.ascontiguousarray(inputs["w_out"][0], np.float32)
    for nm in ("w_gate", "w_up"):
        w = np.asarray(inputs[nm][0], np.float32)
        m[nm] = np.ascontiguousarray(w.reshape(8, 128, 11, 256).transpose(2, 1, 0, 3)).reshape(11, 128, 2048)
    m["w_down"] = np.ascontiguousarray(inputs["w_down"][0], np.float32)
    m["gn"] = np.ascontiguousarray(np.asarray(inputs["ret_gn_gain"][0], np.float32).reshape(4, 128).T)
    lnp = np.stack([inputs["ln1_g"][0], inputs["ln1_b"][0], inputs["ln2_g"][0], inputs["ln2_b"][0]]).astype(np.float32)
    m["lnp"] = np.ascontiguousarray(np.broadcast_to(lnp[None], (128, 4, D)))
    T = _tables(NBLK, s)
    for k, v in T.items():
        if k != "cdecay":
            m[k] = v
    return m, own


def build(NBLK=32, debug=False):
    NOWN = NBLK // 2
    TL = NBLK * 256
    TO = NOWN * 256
    NG = TO // 512
    cdecay = _tables(2, 1)["cdecay"]

    nc = bass.Bass("TRN2", target_bir_lowering=False)
    P = Prog(nc)

    def din(name, shape, dt=F32):
        return nc.dram_tensor(name, list(shape), dt, kind="ExternalInput").ap()

    xT = din("xT", [NBLK, 128, 2048])
    xo = din("xo", [TO, D])
    w_in = din("w_in", [D, 3584])
    w_out = din("w_out", [D, D])
    w_gate = din("w_gate", [11, 128, 2048])
    w_up = din("w_up", [11, 128, 2048])
    w_down = din("w_down", [DFF, D])
    gn_d = din("gn", [128, 4])
    lnp_d = din("lnp", [128, 4, D])
    t_dmask = din("t_dmask", [128, 4, 2, 256])
    t_kend = din("t_kend", [128, 2, 512])
    t_qfs = din("t_qfs", [128, 4, 256])
    t_kstat = din("t_kstat", [34, TL], BF16)
    t_qstat = din("t_qstat", [2, 8, 256], BF16)
    t_cb = din("t_cb", [128, 2, 256], BF16)
    t_ident = din("t_ident", [128, 128], BF16)
    t_identf = din("t_identf", [128, 128])
    t_vb = din("t_vb", [128, NOWN, 4, 32])
    t_mb = din("t_mb", [128, NOWN, 8, 32])
    y = nc.dram_tensor("y", [TO, D], F32, kind="ExternalOutput").ap()
    if debug:
        d_mix = nc.dram_tensor("d_mix", [128, 8, TO], BF16, kind="ExternalOutput").ap()
        d_qa = nc.dram_tensor("d_qa", [98, 256], BF16, kind="ExternalOutput").ap()
        d_ka = nc.dram_tensor("d_ka", [98, TL], BF16, kind="ExternalOutput").ap()
        d_gsb = nc.dram_tensor("d_gsb", [128, 256], F32, kind="ExternalOutput").ap()
        d_m8 = nc.dram_tensor("d_m8", [128, 64], F32, kind="ExternalOutput").ap()
        d_mbt = nc.dram_tensor("d_mbt", [128, 192], BF16, kind="ExternalOutput").ap()
        d_va = nc.dram_tensor("d_va", [128, NBLK * 2 * 4 * 65], BF16, kind="ExternalOutput").ap()
        d_ksum = nc.dram_tensor("d_ksum", [64, 32], F32, kind="ExternalOutput").ap()
        d_mtok = nc.dram_tensor("d_mtok", [128, 512], BF16, kind="ExternalOutput").ap()
        d_h1 = nc.dram_tensor("d_h1", [TO, D], F32, kind="ExternalOutput").ap()
        d_u = nc.dram_tensor("d_u", [TO, D], F32, kind="ExternalOutput").ap()
        d_mv = nc.dram_tensor("d_mv", [128, 8], F32, kind="ExternalOutput").ap()
        d_rs = nc.dram_tensor("d_rs", [128, 8], F32, kind="ExternalOutput").ap()
        d_pt = nc.dram_tensor("d_pt", [3, 128, 512], BF16, kind="ExternalOutput").ap()
        d_rden = nc.dram_tensor("d_rden", [128, 4], F32, kind="ExternalOutput").ap()
    scr_g = nc.dram_tensor("scr_g", [11, 128, 2048], BF16).ap()
    scr_u = nc.dram_tensor("scr_u", [11, 128, 2048], BF16).ap()
    scr_d = nc.dram_tensor("scr_d", [11, 128, 2048], BF16).ap()
    B_scr = Buf("scr")

    w_in_v = w_in.rearrange("(k p) c -> p k c", p=128)
    w_out_v = w_out.rearrange("(k p) c -> p k c", p=128)
    w_down_v = w_down.rearrange("(f p) c -> p f c", p=128)

    top = ExitStack()
    with top:
        def sb(es, name, shape, dt):
            return es.enter_context(nc.sbuf_tensor("sb_" + name, list(shape), dt))

        pbank = [top.enter_context(nc.psum_tensor(f"pb{i}", [128, 512], F32)) for i in range(8)]
        PB = [Buf(f"pb{i}", excl=True) for i in range(8)]

        def pv(i):
            return pbank[i]

        def pv16(i):
            return pbank[i][:, :].bitcast(BF16)

        class Rot:
            def __init__(self, ids):
                self.ids = list(ids)
                self.i = 0

            def next(self):
                b = self.ids[self.i % len(self.ids)]
                self.i += 1
                return b

        def mm(out, lhsT, rhs, start, stop, reads, writes, **kw):
            P.op("pe", lambda e: e.matmul(out, lhsT, rhs, start=start, stop=stop, **kw), reads, writes)

        def tr(out, in_, ident, reads, writes):
            P.op("pe", lambda e: e.transpose(out, in_, ident), reads, writes)

        def act(out, in_, func, reads, writes, **kw):
            P.op("act", lambda e: e.activation(out, in_, func, **kw), reads, writes)

        def v_tt(eng, out, in0, in1, op, reads, writes):
            P.op(eng, lambda e: e.tensor_tensor(out, in0, in1, op), reads, writes)

        def v_ts(eng, out, in0, s1, s2, op0, reads, writes, op1=ALU.bypass):
            P.op(eng, lambda e: e.tensor_scalar(out, in0, s1, s2, op0, op1), reads, writes)

        def v_cp(eng, out, in_, reads, writes):
            P.op(eng, lambda e: e.tensor_copy(out, in_), reads, writes)

        def v_stt(out, in0, scalar, in1, op0, op1, reads, writes):
            P.op("dve", lambda e: e.scalar_tensor_tensor(out, in0, scalar, in1, op0, op1), reads, writes)

        mixM = sb(top, "mixM", [128, 4, TO], BF16)
        B_mixM = Buf("mixM")
        ident = sb(top, "ident", [128, 128], BF16)
        B_ident = Buf("ident")
        P.dma("sp", ident[:, :], t_ident, writes=[B_ident])
        xTc = [sb(top, f"xTc{i}", [128, 8, 256], BF16) for i in range(2)]
        B_xTc = [Buf(f"xTc{i}") for i in range(2)]

        def load_xT(c):
            P.dma("pool", xTc[c % 2][:, :, :].rearrange("p k t -> p (k t)"), xT[c], writes=[B_xTc[c % 2]])

        with ExitStack() as es:
            wMs = [sb(es, f"wM{i}", [128, 8, 768], BF16) for i in range(2)]
            B_wMs = [[Buf(f"wM{i}_{k}") for k in range(3)] for i in range(2)]

            def load_wM_seg(g, seg):
                d0, s0 = [(0, 2048 + 256 * g), (256, 2560 + 256 * g), (512, 3072 + 256 * g)][seg]
                P.dma("pool", wMs[g][:, :, d0:d0 + 256], w_in_v[:, :, s0:s0 + 256], writes=[B_wMs[g][seg]])

            def load_wM(g):
                for seg in (1, 2, 0):
                    load_wM_seg(g, seg)
            KA = [sb(es, f"KA{h}", [98, TL], BF16) for h in range(4)]
            B_KA = [Buf(f"KA{h}") for h in range(4)]
            VA = sb(es, "VA", [128, NBLK * 2, 4, 65], BF16)
            B_VA = Buf("VA")
            ksum = [sb(es, f"ksum{h}", [64, 32], F32) for h in range(4)]
            B_ksum = [Buf(f"ksum{h}") for h in range(4)]
            kmT = [sb(es, f"kmT{h}", [64, 32], BF16) for h in range(4)]
            B_kmT = [Buf(f"kmT{h}") for h in range(4)]
            QA = [[sb(es, f"QA{h}_{i}", [98, 256], BF16) for i in range(2)] for h in range(4)]
            B_QA = [[Buf(f"QA{h}_{i}") for i in range(2)] for h in range(4)]
            gsb = sb(es, "gsb", [128, 2, 4, 32], F32)
            m8 = sb(es, "m8", [128, 2, 4, 8], F32)
            selb = sb(es, "selb", [128, 2, 4, 32], F32)
            B_gate = [Buf("gate0"), Buf("gate1")]
            MBt = [sb(es, f"MBt{i}", [128, 192], BF16) for i in range(2)]
            B_MBt = [Buf("MBt0"), Buf("MBt1")]
            PT = [sb(es, f"PT{i}", [128, 512], BF16) for i in range(3)]
            B_PT = [Buf(f"PT{i}") for i in range(3)]
            mtok = sb(es, "mtok", [128, 2, 256], BF16)
            B_mtok = Buf("mtok")
            rden = sb(es, "rden", [128, 4], F32)
            B_rden = [Buf(f"rden{i}") for i in range(4)]
            vb = sb(es, "vb", [128, NOWN, 4, 32], F32)
            B_vb = Buf("vb")
            mbt = sb(es, "mbt", [128, NOWN, 4, 32], F32)
            B_mbt = Buf("mbt")
            cb = sb(es, "cb", [128, 2, 256], BF16)
            B_cb = Buf("cb")

            P.dma("sp", cb[:, :, :], t_cb, writes=[B_cb])
            P.dma("sp", vb[:, :, :, :], t_vb, writes=[B_vb])
            for h in range(4):
                P.dma("sp", KA[h][64:98, :], t_kstat, writes=[B_KA[h]])
            P.op("pool", lambda e: e.memset(VA[:, :, :, 64:65], 1.0), writes=[B_VA])
            for i in range(2):
                P.op("pool", lambda e, i=i: e.memset(MBt[i][:, 0:64], 0.0), writes=[B_MBt[i]])

            SB_ = Rot([0, 1, 2])
            OB_ = Rot([3, 4])
            MB_ = Rot([5, 6, 7])

            load_wM_seg(0, 1)
            load_xT(0)
            load_wM_seg(0, 2)
            load_xT(1)
            load_wM_seg(0, 0)
            for g in range(2):
                wM = wMs[g]
                B_wM = B_wMs[g]
                P.dma("sp", mbt[:, :, :, :], t_mb[:, :, 4 * g:4 * g + 4, :], writes=[B_mbt])
                for h in range(4):
                    P.op("pool", lambda e, h=h: e.memset(ksum[h][:, :], 0.0), writes=[B_ksum[h]])
                    for i in range(2):
                        P.dma("sp", QA[h][i][96:98, :], t_qstat[:, 4 * g + h, :], writes=[B_QA[h][i]])
                def proj_kv(c, g=g, wM=wM, B_wM=B_wM):
                    xi = c % 2
                    for hp in range(2):
                        bk = MB_.next()
                        for hh in range(2):
                            hl = 2 * hp + hh
                            for kt in range(8):
                                mm(pv(bk)[0:64, hh * 256:(hh + 1) * 256], wM[:, kt, 256 + hl * 64:256 + (hl + 1) * 64],
                                   xTc[xi][:, kt, :], kt == 0, kt == 7, [B_wM[1], B_xTc[xi]], [PB[bk]])
                        for hh in range(2):
                            hl = 2 * hp + hh
                            act(KA[hl][0:64, c * 256:(c + 1) * 256], pv(bk)[0:64, hh * 256:(hh + 1) * 256], AF.Copy,
                                [PB[bk]], [B_KA[hl], B_ksum[hl]], accum_out=ksum[hl][:, c:c + 1])
                    bk = MB_.next()
                    for half in range(2):
                        for kt in range(8):
                            mm(pv(bk)[:, half * 256:(half + 1) * 256], xTc[xi][:, kt, half * 128:(half + 1) * 128],
                               wM[:, kt, 512:768], kt == 0, kt == 7, [B_wM[2], B_xTc[xi]], [PB[bk]])
                    v_cp("dve", VA[:, 2 * c:2 * c + 2, :, 0:64],
                         pv(bk)[:, :].rearrange("p (t h d) -> p t h d", t=2, h=4), [PB[bk]], [B_VA])

                def proj_q_gate(c, g=g, wM=wM, B_wM=B_wM):
                    xi = c % 2
                    j = (c - 1) // 2
                    qb = j % 2
                    for hp in range(2):
                        bk = MB_.next()
                        for hh in range(2):
                            hl = 2 * hp + hh
                            for kt in range(8):
                                mm(pv(bk)[0:64, hh * 256:(hh + 1) * 256], wM[:, kt, hl * 64:(hl + 1) * 64],
                                   xTc[xi][:, kt, :], kt == 0, kt == 7, [B_wM[0], B_xTc[xi]], [PB[bk]])
                        for hh in range(2):
                            hl = 2 * hp + hh
                            v_ts("dve", QA[hl][qb][0:64, :], pv(bk)[0:64, hh * 256:(hh + 1) * 256], 0.125, None, ALU.mult,
                                 [PB[bk]], [B_QA[hl][qb]])
                    for hl in range(4):
                        v_ts("dve", kmT[hl][:, :], ksum[hl][:, :], 1.0 / 256.0, None, ALU.mult, [B_ksum[hl]], [B_kmT[hl]])
                    gk = MB_.next()
                    for qh in range(2):
                        for hl in range(4):
                            o0 = (qh * 4 + hl) * 32
                            mm(pv(gk)[:, o0:o0 + 32], QA[hl][qb][0:64, qh * 128:(qh + 1) * 128], kmT[hl][:, :],
                               True, True, [B_QA[hl][qb], B_kmT[hl]], [PB[gk]])
                    for qh in range(2):
                        v_tt("dve", gsb[:, qh, :, :], pv(gk)[:, qh * 128:(qh + 1) * 128].rearrange("p (h n) -> p h n", h=4),
                             vb[:, j, :, :], ALU.add, [PB[gk], B_vb], [B_gate[qh]])
                        for hl in range(4):
                            P.op("dve", lambda e, qh=qh, hl=hl: e.max(m8[:, qh, hl, :], gsb[:, qh, hl, :]),
                                 [], [B_gate[qh]])
                        v_tt("dve", selb[:, qh, :, :], gsb[:, qh, :, :], m8[:, qh, :, 2:3].broadcast_to([128, 4, 32]), ALU.is_ge,
                             [], [B_gate[qh]])
                        v_stt(MBt[qh][:, 64:192].rearrange("p (h n) -> p h n", h=4), selb[:, qh, :, :], BIG,
                              mbt[:, j, :, :], ALU.mult, ALU.add, [B_gate[qh], B_mbt], [B_MBt[qh]])

                def mask_T(c, g=g):
                    j = (c - 1) // 2
                    qb = j % 2
                    tk = MB_.next()
                    for hl in range(4):
                        for qh in range(2):
                            o0 = (hl * 2 + qh) * 128
                            tr(pv16(tk)[0:96, o0:o0 + 128], MBt[qh][:, 32 * hl:32 * hl + 96], ident[:, :],
                               [B_MBt[qh], B_ident], [PB[tk]])
                    for hl in range(4):
                        v_cp("dve", QA[hl][qb][64:96, :], pv16(tk)[64:96, hl * 256:(hl + 1) * 256], [PB[tk]], [B_QA[hl][qb]])

                def attention(c, hook, hook2, g=g):
                    j = (c - 1) // 2
                    qb = j % 2
                    items = [(hl, p_) for hl in range(4) for p_ in range(c + 1)]
                    info = {}
                    LOOK = 2
                    for it in range(len(items) + LOOK):
                        if it == 2 * (c + 1):
                            hook()
                        if it == 3 * (c + 1) + (c + 1) // 2:
                            hook2()
                        if it < len(items):
                            hl, p_ = items[it]
                            sbk = SB_.next()
                            pti = it % 3
                            diag = (p_ == c)
                            for t in range(2):
                                kt = 2 * p_ + t
                                mm(pv(sbk)[:, t * 256:(t + 1) * 256], KA[hl][0:98, kt * 128:(kt + 1) * 128], QA[hl][qb][0:98, :],
                                   True, not diag, [B_KA[hl], B_QA[hl][qb]], [PB[sbk]])
                                if diag:
                                    mm(pv(sbk)[:, t * 256:(t + 1) * 256], ident[:, :], cb[:, t, :], False, True,
                                       [B_ident, B_cb], [PB[sbk]])
                            act(PT[pti][:, :], pv(sbk)[:, :], AF.Exp, [PB[sbk]], [B_PT[pti]])
                            info[it] = pti
                        if it - LOOK >= 0:
                            it2 = it - LOOK
                            hl, p_ = items[it2]
                            pti = info[it2]
                            diag = (p_ == c)
                            if p_ == 0:
                                ob = OB_.next()
                                info[("ob", hl)] = ob
                            ob = info[("ob", hl)]
                            for t in range(2):
                                kt = 2 * p_ + t
                                for qh in range(2):
                                    if diag and t == 1 and qh == 0:
                                        continue
                                    first = (p_ == 0 and t == 0 and qh == 0)
                                    mm(pv(ob)[:, qh * 128:qh * 128 + 65], PT[pti][:, t * 256 + qh * 128:t * 256 + (qh + 1) * 128],
                                       VA[:, kt, hl, :], first, diag and t == 1 and qh == 1, [B_PT[pti], B_VA], [PB[ob]],
                                       skip_group_check=True)
                            if diag:
                                for qh in range(2):
                                    ri = (hl % 2) * 2 + qh
                                    P.op("dve", lambda e, ob=ob, qh=qh, ri=ri: e.reciprocal(rden[:, ri:ri + 1], pv(ob)[:, qh * 128 + 64:qh * 128 + 65]),
                                         [PB[ob]], [B_rden[ri]])
                                    v_ts("dve", mtok[:, qh, hl * 64:(hl + 1) * 64], pv(ob)[:, qh * 128:qh * 128 + 64],
                                         rden[:, ri:ri + 1], None, ALU.mult, [PB[ob], B_rden[ri]], [B_mtok])
                    tk = MB_.next()
                    for pr in range(2):
                        for qh in range(2):
                            o0 = (pr * 2 + qh) * 128
                            tr(pv16(tk)[:, o0:o0 + 128], mtok[:, qh, pr * 128:(pr + 1) * 128], ident[:, :],
                               [B_mtok, B_ident], [PB[tk]])
                    for pr in range(2):
                        v_cp("dve", mixM[:, 2 * g + pr, j * 256:(j + 1) * 256], pv16(tk)[:, pr * 256:(pr + 1) * 256],
                             [PB[tk]], [B_mixM])

                if g == 1:
                    load_xT(0)
                    load_xT(1)
                proj_kv(0)
                proj_kv(1)
                proj_q_gate(1)
                mask_T(1)
                for c in range(1, NBLK, 2):
                    if g == 0 and c == (NBLK // 2) + 1:
                        load_wM(1)
                    if c + 2 < NBLK:
                        load_xT(c + 1)
                        load_xT(c + 2)

                        def hook(c=c):
                            proj_kv(c + 1)
                            proj_kv(c + 2)
                            proj_q_gate(c + 2)

                        def hook2(c=c):
                            mask_T(c + 2)
                    else:
                        def hook():
                            pass

                        def hook2():
                            pass
                    attention(c, hook, hook2)
            if debug:
                qb_last = ((NBLK - 2) // 2) % 2
                P.dma("sp", d_qa, QA[0][qb_last][:, :], reads=[B_QA[0][qb_last]])
                P.dma("sp", d_ka, KA[0][:, :], reads=[B_KA[0]])
                P.dma("sp", d_gsb, gsb[:, :, :, :].rearrange("p a h n -> p (a h n)"), reads=[B_gate[0], B_gate[1]])
                P.dma("sp", d_m8, m8[:, :, :, :].rearrange("p a h n -> p (a h n)"), reads=[B_gate[0], B_gate[1]])
                P.dma("sp", d_mbt, MBt[0][:, :], reads=[B_MBt[0]])
                P.dma("sp", d_va, VA[:, :, :, :].rearrange("p a h n -> p (a h n)"), reads=[B_VA])
                P.dma("sp", d_ksum, ksum[0][:, :], reads=[B_ksum[0]])
                P.dma("sp", d_mtok, mtok[:, :, :].rearrange("p a n -> p (a n)"), reads=[B_mtok])
                for i in range(3):
                    P.dma("sp", d_pt[i], PT[i][:, :], reads=[B_PT[i]])
                P.dma("sp", d_rden, rden[:, :], reads=B_rden)
            P.barrier()

        with ExitStack() as es2:
            mixR = sb(es2, "mixR", [128, 4, TO], BF16)
            B_mixR = Buf("mixR")

            with ExitStack() as es:
                wR = sb(es, "wR", [128, 8, 2048], BF16)
                B_wRs = [Buf(f"wR{k}") for k in range(4)]
                dmask = sb(es, "dmask", [128, 4, 2, 256], F32)
                kend = sb(es, "kend", [128, 2, 512], F32)
                qfs = sb(es, "qfs", [128, 4, 256], F32)
                gn = sb(es, "gn", [128, 4], F32)
                B_tab = Buf("tabC")
                onesd = sb(es, "onesd", [128, 128], F32)
                B_ones = Buf("onesd")
                stg = [sb(es, f"stg{i}", [128, 2048], BF16) for i in range(2)]
                B_stg = [Buf(f"stg{i}") for i in range(2)]
                B_stgo = [Buf(f"stgo{i}") for i in range(2)]
                kT = [sb(es, f"kT{i}", [128, 4, 256], BF16) for i in range(2)]
                B_kT = [Buf(f"kT{i}") for i in range(2)]
                ktok = [sb(es, f"ktok{i}", [128, 2, 512], BF16) for i in range(2)]
                B_ktok = [Buf(f"ktok{i}") for i in range(2)]
                vtok = [sb(es, f"vtok{i}", [128, 2, 512], BF16) for i in range(2)]
                B_vtok = [Buf(f"vtok{i}") for i in range(2)]
                state = sb(es, "state", [128, 4, 128], F32)
                B_state = Buf("state")
                stbf = [sb(es, f"stbf{i}", [128, 4, 128], BF16) for i in range(2)]
                B_stbf = [Buf(f"stbf{i}") for i in range(2)]
                qT = [sb(es, f"qT{i}", [128, 4, 256], BF16) for i in range(2)]
                B_qT = [Buf(f"qT{i}") for i in range(2)]
                qsT = [sb(es, f"qsT{i}", [128, 4, 256], BF16) for i in range(2)]
                B_qsT = [Buf(f"qsT{i}") for i in range(2)]
                gsil = [sb(es, f"gsil{i}", [128, 4, 256], F32) for i in range(2)]
                B_gsil = [Buf(f"gsil{i}") for i in range(2)]
                sT = [sb(es, f"sT{i}", [128, 4, 2, 256], BF16) for i in range(2)]
                B_sT = [Buf(f"sT{i}") for i in range(2)]
                gnt = [[sb(es, f"gnt{i}_{k}", [128, 256], F32) for k in range(6)] for i in range(4)]
                B_gnt = [[Buf(f"gnt{i}_{k}") for k in range(6)] for i in range(4)]

                P.dma("sp", dmask[:, :, :, :], t_dmask, writes=[B_tab])
                P.dma("sp", kend[:, :, :], t_kend, writes=[B_tab])
                P.dma("sp", qfs[:, :, :], t_qfs, writes=[B_tab])
                P.dma("sp", gn[:, :], gn_d, writes=[B_tab])
                P.op("pool", lambda e: e.memset(onesd[:, :], 1.0 / 128.0), writes=[B_ones])
                P.op("pool", lambda e: e.memset(state[:, :, :], 0.0), writes=[B_state])
                for q4 in (1, 2):
                    P.dma("pool", wR[:, :, q4 * 512:(q4 + 1) * 512], w_in_v[:, :, q4 * 512:(q4 + 1) * 512], writes=[B_wRs[q4]])
                load_xT(0)
                for q4 in (0, 3):
                    P.dma("pool", wR[:, :, q4 * 512:(q4 + 1) * 512], w_in_v[:, :, q4 * 512:(q4 + 1) * 512], writes=[B_wRs[q4]])

                precast = []
                for i in range(11):
                    precast.append((scr_g[i], w_gate[i], "pkc"))
                    precast.append((scr_u[i], w_up[i], "pkc"))
                    precast.append((scr_d[i], w_down_v[:, 2 * i:2 * i + 2, :], "pfc"))
                pc_i = [0]

                def do_precast(n):
                    for _ in range(n):
                        if pc_i[0] >= len(precast):
                            return
                        dst, src, kind = precast[pc_i[0]]
                        si = pc_i[0] % 2
                        pc_i[0] += 1
                        if kind == "pkc":
                            P.dma("pool", stg[si][:, :], src, writes=[B_stg[si]])
                        else:
                            P.dma("pool", stg[si][:, :].rearrange("p (f c) -> p f c", f=2), src, writes=[B_stg[si]])
                        P.dma("sp", dst, stg[si][:, :], reads=[B_stg[si]], writes=[Buf("scrchunk")], semb=B_stgo[si])

                RB = Rot(list(range(8)))
                pending = [None]
                pendA = [None]
                for c in range(NBLK):
                    xi = c % 2
                    bi = c % 2
                    if c + 1 < NBLK:
                        load_xT(c + 1)
                    do_precast(2 if c < 8 else 1)
                    own = (c % 2 == 1)
                    j = (c - 1) // 2
                    oi = j % 2
                    if own:
                        for hp in range(2):
                            bk = RB.next()
                            for hh in range(2):
                                h = 2 * hp + hh
                                for kt in range(8):
                                    mm(pv(bk)[:, hh * 256:(hh + 1) * 256], wR[:, kt, 512 + h * 128:512 + (h + 1) * 128],
                                       xTc[xi][:, kt, :], kt == 0, kt == 7, [B_wRs[1], B_xTc[xi]], [PB[bk]])
                            act(kT[bi][:, 2 * hp:2 * hp + 2, :], pv(bk)[:, :].rearrange("p (h t) -> p h t", h=2), AF.Copy,
                                [PB[bk]], [B_kT[bi]], scale=128.0 ** -0.5)
                        for hp in range(2):
                            bk = RB.next()
                            for hh in range(2):
                                h = 2 * hp + hh
                                for kt in range(8):
                                    mm(pv(bk)[:, hh * 256:(hh + 1) * 256], wR[:, kt, h * 128:(h + 1) * 128],
                                       xTc[xi][:, kt, :], kt == 0, kt == 7, [B_wRs[0], B_xTc[xi]], [PB[bk]])
                            v_cp("dve", qT[oi][:, 2 * hp:2 * hp + 2, :], pv(bk)[:, :].rearrange("p (h t) -> p h t", h=2),
                                 [PB[bk]], [B_qT[oi]])
                            v_tt("dve", qsT[oi][:, 2 * hp:2 * hp + 2, :], pv(bk)[:, :].rearrange("p (h t) -> p h t", h=2),
                                 qfs[:, 2 * hp:2 * hp + 2, :], ALU.mult, [PB[bk], B_tab], [B_qsT[oi]])
                    for half in range(2):
                        bk = RB.next()
                        for kt in range(8):
                            mm(pv(bk)[:, :], xTc[xi][:, kt, half * 128:(half + 1) * 128], wR[:, kt, 512:1024],
                               kt == 0, kt == 7, [B_wRs[1], B_xTc[xi]], [PB[bk]])
                        v_tt("dve", ktok[bi][:, half, :], pv(bk)[:, :], kend[:, half, :], ALU.mult,
                             [PB[bk], B_tab], [B_ktok[bi]])
                    for half in range(2):
                        bk = RB.next()
                        for kt in range(8):
                            mm(pv(bk)[:, :], xTc[xi][:, kt, half * 128:(half + 1) * 128], wR[:, kt, 1024:1536],
                               kt == 0, kt == 7, [B_wRs[2], B_xTc[xi]], [PB[bk]])
                        act(vtok[bi][:, half, :], pv(bk)[:, :], AF.Copy, [PB[bk]], [B_vtok[bi]])
                    if pending[0] is not None:
                        pending[0]()
                        pending[0] = None
                    if pendA[0] is not None:
                        pendA[0]()
                        pendA[0] = None
                    if own:
                        for hp in range(2):
                            bk = RB.next()
                            for hh in range(2):
                                h = 2 * hp + hh
                                for kt in range(8):
                                    mm(pv(bk)[:, hh * 256:(hh + 1) * 256], wR[:, kt, 1536 + h * 128:1536 + (h + 1) * 128],
                                       xTc[xi][:, kt, :], kt == 0, kt == 7, [B_wRs[3], B_xTc[xi]], [PB[bk]])
                            act(gsil[oi][:, 2 * hp:2 * hp + 2, :], pv(bk)[:, :].rearrange("p (h t) -> p h t", h=2), AF.Silu,
                                [PB[bk]], [B_gsil[oi]])
                        v_cp("pool", stbf[oi][:, :, :], state[:, :, :], [B_state], [B_stbf[oi]])
                    bk = RB.next()
                    for h in range(4):
                        for half in range(2):
                            mm(pv(bk)[:, h * 128:(h + 1) * 128], ktok[bi][:, half, h * 128:(h + 1) * 128],
                               vtok[bi][:, half, h * 128:(h + 1) * 128], half == 0, half == 1,
                               [B_ktok[bi], B_vtok[bi]], [PB[bk]])
                    for h in range(4):
                        v_stt(state[:, h, :], state[:, h, :], cdecay[h], pv(bk)[:, h * 128:(h + 1) * 128], ALU.mult, ALU.add,
                              [PB[bk]], [B_state])
                    if not own:
                        continue
                    def gn_chain(j=j, oi=oi):
                        for h in range(4):
                            act(gnt[h][4][:, :], gnt[h][3][:, :], AF.Sqrt, [B_gnt[h][3]], [B_gnt[h][4]])
                        for h in range(4):
                            P.op("dve", lambda e, h=h: e.reciprocal(gnt[h][4][:, :], gnt[h][4][:, :]), [], [B_gnt[h][4]])
                        for h in range(4):
                            v_tt("pool", gnt[h][5][:, :], gnt[h][5][:, :], gnt[h][4][:, :], ALU.mult, [B_gnt[h][4]], [B_gnt[h][5]])
                        for h in range(4):
                            v_stt(mixR[:, h, j * 256:(j + 1) * 256], gnt[h][5][:, :], gn[:, h:h + 1], gsil[oi][:, h, :], ALU.mult, ALU.mult,
                                  [B_gnt[h][5], B_tab, B_gsil[oi]], [B_mixR])

                    def part_a(j=j, oi=oi, bi=bi, chain=gn_chain):
                        for h in range(4):
                            bk = RB.next()
                            for jh in range(2):
                                mm(pv(bk)[:, jh * 256:(jh + 1) * 256], kT[bi][:, h, jh * 128:(jh + 1) * 128], qT[oi][:, h, :],
                                   True, True, [B_kT[bi], B_qT[oi]], [PB[bk]])
                            v_tt("dve", sT[oi][:, h, :, :], pv(bk)[:, :].rearrange("p (a t) -> p a t", a=2), dmask[:, h, :, :],
                                 ALU.mult, [PB[bk], B_tab], [B_sT[oi]])
                        rbk = [RB.next(), RB.next()]
                        for h in range(4):
                            bk = rbk[h // 2]
                            o0 = (h % 2) * 256
                            for jh in range(2):
                                mm(pv(bk)[:, o0:o0 + 256], vtok[bi][:, jh, h * 128:(h + 1) * 128], sT[oi][:, h, jh, :], jh == 0, False,
                                   [B_vtok[bi], B_sT[oi]], [PB[bk]])
                            mm(pv(bk)[:, o0:o0 + 256], stbf[oi][:, h, :], qsT[oi][:, h, :], False, True, [B_stbf[oi], B_qsT[oi]], [PB[bk]])
                        for h in range(4):
                            bk = rbk[h // 2]
                            o0 = (h % 2) * 256
                            act(gnt[h][0][:, :], pv(bk)[:, o0:o0 + 256], AF.Copy, [PB[bk]], [B_gnt[h][0]])
                            act(gnt[h][1][:, :], pv(bk)[:, o0:o0 + 256], AF.Square, [PB[bk]], [B_gnt[h][1]])
                        sbs = []
                        for h in range(4):
                            bs = RB.next()
                            sbs.append(bs)
                            mm(pv(bs)[:, 0:256], onesd[:, :], gnt[h][0][:, :], True, True, [B_ones, B_gnt[h][0]], [PB[bs]])
                            mm(pv(bs)[:, 256:512], onesd[:, :], gnt[h][1][:, :], True, True, [B_ones, B_gnt[h][1]], [PB[bs]])
                        for h in range(4):
                            act(gnt[h][2][:, :], pv(sbs[h])[:, 0:256], AF.Square, [PB[sbs[h]]], [B_gnt[h][2]])
                        for h in range(4):
                            v_stt(gnt[h][3][:, :], pv(sbs[h])[:, 256:512], EPS, gnt[h][2][:, :], ALU.add, ALU.subtract,
                                  [PB[sbs[h]], B_gnt[h][2]], [B_gnt[h][3]])
                            v_tt("dve", gnt[h][5][:, :], gnt[h][0][:, :], pv(sbs[h])[:, 0:256], ALU.subtract,
                                 [B_gnt[h][0], PB[sbs[h]]], [B_gnt[h][5]])
                        pending[0] = chain

                    pendA[0] = part_a
                if pendA[0] is not None:
                    pendA[0]()
                    pendA[0] = None
                if pending[0] is not None:
                    pending[0]()
                    pending[0] = None
                do_precast(100)
                P.barrier()

            if debug:
                P.dma("sp", d_mix[:, 0:4, :], mixR[:, :, :], reads=[B_mixR])
                P.dma("sp", d_mix[:, 4:8, :], mixM[:, :, :], reads=[B_mixM])

            with ExitStack() as es:
                wo = sb(es, "wo", [128, 8, D], BF16)
                B_wo = Buf("wo")
                lnp = sb(es, "lnp", [128, 4, D], F32)
                B_lnp = Buf("lnp")
                identf = sb(es, "identf", [128, 128], F32)
                B_identf = Buf("identf")
                wg = [sb(es, f"wg{i}", [128, 8, 256], BF16) for i in range(2)]
                wu = [sb(es, f"wu{i}", [128, 8, 256], BF16) for i in range(2)]
                B_wg = [Buf(f"wg{i}") for i in range(2)]
                B_wu = [Buf(f"wu{i}") for i in range(2)]
                NWD = 6
                wd = [sb(es, f"wd{i}", [128, 2, 512], BF16) for i in range(NWD)]
                B_wd = [Buf(f"wd{i}") for i in range(NWD)]
                h1 = [sb(es, f"h1_{i}", [128, 4, D], F32) for i in range(2)]
                B_h1 = [[Buf(f"h1_{i}_{t}") for t in range(4)] for i in range(2)]
                h1T = sb(es, "h1T", [128, 8, 512], BF16)
                B_h1T = Buf("h1T")
                hidT = sb(es, "hidT", [128, NFT, 512], BF16)
                B_hidT = Buf("hidT")
                xot = [sb(es, f"xot{i}", [128, D], F32) for i in range(2)]
                B_xot = [Buf(f"xot{i}") for i in range(2)]
                gs = [sb(es, f"gs{i}", [128, 512], BF16) for i in range(4)]
                B_gs = [Buf(f"gs{i}") for i in range(4)]
                st6 = sb(es, "st6", [128, 8, 2, 6], F32)
                mv = sb(es, "mv", [128, 8, 2], F32)
                rs = sb(es, "rs", [128, 8, 2], F32)
                B_st = [Buf(f"st{i}") for i in range(8)]

                for k2 in range(4):
                    P.dma("pool", wo[:, 2 * k2:2 * k2 + 2, :], w_out_v[:, 2 * k2:2 * k2 + 2, :], writes=[B_wo])
                P.dma("sp", lnp[:, :, :], lnp_d, writes=[B_lnp])
                P.dma("sp", identf[:, :], t_identf, writes=[B_identf])
                DB = Rot(list(range(8)))
                xo_i = [0]
                scr_d_v = [scr_d[i].rearrange("p (f c) -> p f c", f=2) for i in range(11)]

                def layer_norm4(hb, gi):
                    for tt in range(4):
                        si = hb * 4 + tt
                        for a_ in range(2):
                            P.op("dve", lambda e, a_=a_, si=si, tt=tt: e.bn_stats(st6[:, si, a_, :], h1[hb][:, tt, a_ * 512:(a_ + 1) * 512]),
                                 [B_h1[hb][tt]], [B_st[si]])
                        P.op("dve", lambda e, si=si: e.bn_aggr(mv[:, si, :], st6[:, si, :, :].rearrange("p a b -> p (a b)")), [], [B_st[si]])
                        v_ts("dve", rs[:, si, 0:1], mv[:, si, 1:2], EPS, None, ALU.add, [], [B_st[si]])
                    for tt in range(4):
                        si = hb * 4 + tt
                        act(rs[:, si, 0:1], rs[:, si, 0:1], AF.Sqrt, [], [B_st[si]])
                    for tt in range(4):
                        si = hb * 4 + tt
                        P.op("dve", lambda e, si=si: e.reciprocal(rs[:, si, 1:2], rs[:, si, 0:1]), [], [B_st[si]])
                    for tt in range(4):
                        si = hb * 4 + tt
                        u = h1[hb][:, tt, :]
                        v_ts("dve", u, u, mv[:, si, 0:1], rs[:, si, 1:2], ALU.subtract, [B_st[si]], [B_h1[hb][tt]], op1=ALU.mult)
                    for tt in range(4):
                        u = h1[hb][:, tt, :]
                        v_tt("pool", u, u, lnp[:, gi, :], ALU.mult, [B_lnp], [B_h1[hb][tt]])
                        v_tt("pool", u, u, lnp[:, gi + 1, :], ALU.add, [B_lnp], [B_h1[hb][tt]])

                def layer_norm1(hb, tt, gi):
                    si = hb * 4 + tt
                    u = h1[hb][:, tt, :]
                    Bh = B_h1[hb][tt]
                    for a_ in range(2):
                        P.op("dve", lambda e, a_=a_: e.bn_stats(st6[:, si, a_, :], h1[hb][:, tt, a_ * 512:(a_ + 1) * 512]), [Bh], [B_st[si]])
                    P.op("dve", lambda e: e.bn_aggr(mv[:, si, :], st6[:, si, :, :].rearrange("p a b -> p (a b)")), [], [B_st[si]])
                    v_ts("dve", rs[:, si, 0:1], mv[:, si, 1:2], EPS, None, ALU.add, [], [B_st[si]])
                    act(rs[:, si, 0:1], rs[:, si, 0:1], AF.Sqrt, [], [B_st[si]])
                    P.op("dve", lambda e: e.reciprocal(rs[:, si, 1:2], rs[:, si, 0:1]), [], [B_st[si]])
                    v_ts("dve", u, u, mv[:, si, 0:1], rs[:, si, 1:2], ALU.subtract, [B_st[si]], [Bh], op1=ALU.mult)
                    v_tt("pool", u, u, lnp[:, gi, :], ALU.mult, [B_lnp], [Bh])
                    v_tt("pool", u, u, lnp[:, gi + 1, :], ALU.add, [B_lnp], [Bh])

                def load_gu(fc):
                    wi = fc % 2
                    P.dma("sp", wg[wi][:, :, :].rearrange("p k c -> p (k c)"), scr_g[fc], writes=[B_wg[wi]])
                    P.dma("sp", wu[wi][:, :, :].rearrange("p k c -> p (k c)"), scr_u[fc], writes=[B_wu[wi]])

                def load_wd(i):
                    half, fc = divmod(i, 11)
                    P.dma("sp", wd[i % NWD][:, :, :], scr_d_v[fc][:, :, half * 512:(half + 1) * 512], writes=[B_wd[i % NWD]])

                def stage1(G):
                    hb = G % 2
                    for tt in range(4):
                        tok0 = G * 512 + tt * 128
                        xb_ = xo_i[0] % 2
                        xo_i[0] += 1
                        P.dma("sp", xot[xb_][:, :], xo[tok0:tok0 + 128, :], writes=[B_xot[xb_]])
                        for half in range(2):
                            bk = DB.next()
                            for kt in range(8):
                                src = mixR if kt < 4 else mixM
                                Bsrc = B_mixR if kt < 4 else B_mixM
                                mm(pv(bk)[:, :], src[:, kt % 4, tok0:tok0 + 128], wo[:, kt, half * 512:(half + 1) * 512],
                                   kt == 0, kt == 7, [Bsrc, B_wo], [PB[bk]])
                            v_stt(h1[hb][:, tt, half * 512:(half + 1) * 512], xot[xb_][:, half * 512:(half + 1) * 512], ALPHA,
                                  pv(bk)[:, :], ALU.mult, ALU.add, [B_xot[xb_], PB[bk]], [B_h1[hb][tt]])
                    layer_norm4(hb, 0)
                    if debug:
                        for tt in range(4):
                            tok0 = G * 512 + tt * 128
                            P.dma("sp", d_h1[tok0:tok0 + 128, :], h1[hb][:, tt, :], reads=[B_h1[hb][tt]])

                def stage2a(G):
                    hb = G % 2
                    for tt in range(4):
                        for k4 in range(2):
                            bk = DB.next()
                            for kk in range(4):
                                kt = k4 * 4 + kk
                                tr(pv(bk)[:, kk * 128:(kk + 1) * 128], h1[hb][:, tt, kt * 128:(kt + 1) * 128], identf[:, :],
                                   [B_h1[hb][tt], B_identf], [PB[bk]])
                            act(h1T[:, k4 * 4:k4 * 4 + 4, tt * 128:(tt + 1) * 128], pv(bk)[:, :].rearrange("p (k t) -> p k t", k=4),
                                AF.Copy, [PB[bk]], [B_h1T])

                def stage2b(G, ln2_of=None):
                    for fc in range(11):
                        wi = fc % 2
                        if ln2_of is not None and fc in (2, 4, 6, 8):
                            stage3b_tile(ln2_of, (fc - 2) // 2)
                        for fi in range(2):
                            ft = fc * 2 + fi
                            bg = DB.next()
                            bu = DB.next()
                            for kt in range(8):
                                mm(pv(bg)[:, :], wg[wi][:, kt, fi * 128:(fi + 1) * 128], h1T[:, kt, :], kt == 0, kt == 7,
                                   [B_wg[wi], B_h1T], [PB[bg]])
                            for kt in range(8):
                                mm(pv(bu)[:, :], wu[wi][:, kt, fi * 128:(fi + 1) * 128], h1T[:, kt, :], kt == 0, kt == 7,
                                   [B_wu[wi], B_h1T], [PB[bu]])
                            gi_ = ft % 4
                            act(gs[gi_][:, :], pv(bg)[:, :], AF.Silu, [PB[bg]], [B_gs[gi_]])
                            v_tt("dve", hidT[:, ft, :], gs[gi_][:, :], pv(bu)[:, :], ALU.mult, [B_gs[gi_], PB[bu]], [B_hidT])
                        if fc + 2 < 11:
                            load_gu(fc + 2)
                    for i in range(NWD):
                        load_wd(i)
                    if G + 1 < NG:
                        load_gu(0)
                        load_gu(1)

                def stage3a(G):
                    hb = G % 2
                    for half in range(2):
                        banks = [DB.next() for _ in range(4)]
                        for fc in range(11):
                            i = half * 11 + fc
                            for fi in range(2):
                                ft = fc * 2 + fi
                                for tt in range(4):
                                    mm(pv(banks[tt])[:, :], hidT[:, ft, tt * 128:(tt + 1) * 128], wd[i % NWD][:, fi, :],
                                       ft == 0, ft == NFT - 1, [B_hidT, B_wd[i % NWD]], [PB[banks[tt]]])
                            if i + NWD < 22:
                                load_wd(i + NWD)
                        for tt in range(4):
                            v_stt(h1[hb][:, tt, half * 512:(half + 1) * 512], h1[hb][:, tt, half * 512:(half + 1) * 512], ALPHA,
                                  pv(banks[tt])[:, :], ALU.mult, ALU.add, [PB[banks[tt]]], [B_h1[hb][tt]])

                def stage3b_tile(G, tt):
                    hb = G % 2
                    tok0 = G * 512 + tt * 128
                    layer_norm1(hb, tt, 2)
                    P.dma("sp", y[tok0:tok0 + 128, :], h1[hb][:, tt, :], reads=[B_h1[hb][tt]])

                def stage3b(G):
                    hb = G % 2
                    layer_norm4(hb, 2)
                    for tt in range(4):
                        tok0 = G * 512 + tt * 128
                        P.dma("sp", y[tok0:tok0 + 128, :], h1[hb][:, tt, :], reads=[B_h1[hb][tt]])

                load_gu(0)
                load_gu(1)
                stage1(0)
                stage2a(0)
                stage2b(0)
                for G in range(NG):
                    if G + 1 < NG:
                        stage1(G + 1)
                    stage3a(G)
                    if G + 1 < NG:
                        stage2a(G + 1)
                        stage2b(G + 1, ln2_of=G)
                    else:
                        stage3b(G)
                P.barrier()

    P.emit()
    return nc


_CACHE = {}


def run(inputs, NBLK=32, debug=False, cores=None, trace=False):
    B = np.asarray(inputs["x"]).shape[0]
    key = (NBLK, debug)
    if key not in _CACHE:
        _CACHE[key] = build(NBLK, debug)
    nc = _CACHE[key]
    plan = [(b, s) for b in range(B) for s in range(2)]
    if cores is not None:
        plan = plan[:cores]
    in_maps, owns = [], []
    for (b, s) in plan:
        m, own = _core_inputs(inputs, b, s, NBLK)
        in_maps.append(m)
        owns.append(own)
    res = run_bass_kernel_spmd(nc, in_maps, core_ids=list(range(len(plan))), **({"trace": True} if trace else {}))
    S = NBLK * 256
    out = np.zeros((B, S, D), np.float32)
    dbg = {}
    for ci, (b, s) in enumerate(plan):
        yo = np.asarray(res.results[ci]["y"], np.float32)
        for j, g in enumerate(owns[ci]):
            out[b, g * 256:(g + 1) * 256] = yo[j * 256:(j + 1) * 256]
        if debug:
            dbg[(b, s)] = (np.asarray(res.results[ci]["d_mix"]), owns[ci], {k: np.asarray(v) for k, v in res.results[ci].items()})
    return out, dbg, res


def kernel(x, w_in, ret_gn_gain, w_out, ln1_g, ln1_b, w_gate, w_up, w_down, ln2_g, ln2_b):
    inputs = dict(x=x, w_in=w_in, ret_gn_gain=ret_gn_gain, w_out=w_out, ln1_g=ln1_g, ln1_b=ln1_b,
                  w_gate=w_gate, w_up=w_up, w_down=w_down, ln2_g=ln2_g, ln2_b=ln2_b)
    out, _, _ = run(inputs, NBLK=32)
    return out
```
